# Optimizing a Trainium2 kernel written in Bass

```python
import jax, jax.numpy as jnp
from jax import lax
import numpy as np

D_MODEL = 1024
BATCH = 2
SEQ = 8192
DEPTH = 4

CONV_WIDTH = 3
CONV_DIM = D_MODEL // 2
CONV_GROUPS = 8
MLSTM_HEADS = 4
MLSTM_HEAD_DIM = (D_MODEL // 2) // MLSTM_HEADS
MLSTM_DIM = MLSTM_HEADS * MLSTM_HEAD_DIM
MLSTM_CHUNK = 128
SGU_CHUNK = 128
SGU_DIM = D_MODEL
SGU_GROUPS = 8
SGU_GROUP_DIM = SGU_DIM // SGU_GROUPS
D_FF = ((8 * D_MODEL // 3 + 255) // 256) * 256
RMS_EPS = 1e-6
EVEN_IN = 3 * CONV_DIM + 4 * MLSTM_DIM + 2 * MLSTM_HEADS
N_EVEN = (DEPTH + 1) // 2
N_ODD = DEPTH // 2

kernel_name = "hybrid_shortconv_mlstm_sgu_trunk"


def rmsnorm(x, g):
    xf = x.astype(jnp.float32)
    y = xf * lax.rsqrt(jnp.mean(xf * xf, axis=-1, keepdims=True) + RMS_EPS)
    return (y * g.astype(jnp.float32)).astype(x.dtype)


def causal_short_conv(z, w):
    S = z.shape[1]
    zp = jnp.pad(z, ((0, 0), (CONV_WIDTH - 1, 0), (0, 0)))
    out = zp[:, 0:S] * w[0]
    for j in range(1, CONV_WIDTH):
        out = out + zp[:, j:j + S] * w[j]
    return out


def mlstm_chunkwise(q, k, v, i_raw, f_raw):
    Bsz, S, H, Dh = q.shape
    L = MLSTM_CHUNK
    nc = S // L
    f32 = jnp.float32

    def chunks4(a):
        return a.astype(f32).reshape(Bsz, nc, L, H, Dh).transpose(1, 0, 3, 2, 4)

    def chunks3(a):
        return a.astype(f32).reshape(Bsz, nc, L, H).transpose(1, 0, 3, 2)

    qc = chunks4(q)
    kc = chunks4(k) * (Dh ** -0.5)
    vc = chunks4(v)
    ic = chunks3(i_raw)
    lfc = chunks3(jax.nn.log_sigmoid(f_raw.astype(f32)))
    causal = jnp.tril(jnp.ones((L, L), dtype=bool))

    def step(carry, xs):
        C, n, m = carry
        qb, kb, vb, ib, lfb = xs
        b = jnp.cumsum(lfb, axis=-1)
        dmat = jnp.where(causal, b[..., :, None] - b[..., None, :] + ib[..., None, :], -jnp.inf)
        m_inter = b + m[..., None]
        m_t = jnp.maximum(m_inter, jnp.max(dmat, axis=-1))
        scores = jnp.einsum('bhtd,bhsd->bhts', qb, kb) * jnp.exp(dmat - m_t[..., None])
        inter = jnp.exp(m_inter - m_t)
        num = (jnp.einsum('bhts,bhsd->bhtd', scores, vb)
               + inter[..., None] * jnp.einsum('bhtd,bhde->bhte', qb, C))
        den = jnp.sum(scores, axis=-1) + inter * jnp.einsum('bhtd,bhd->bht', qb, n)
        h = num / jnp.maximum(jnp.abs(den), jnp.exp(-m_t))[..., None]
        bL = b[..., -1]
        w_end = bL[..., None] - b + ib
        m_new = jnp.maximum(bL + m, jnp.max(w_end, axis=-1))
        decay = jnp.exp(bL + m - m_new)
        wk = jnp.exp(w_end - m_new[..., None])[..., None] * kb
        C_new = decay[..., None, None] * C + jnp.einsum('bhsd,bhse->bhde', wk, vb)
        n_new = decay[..., None] * n + jnp.sum(wk, axis=-2)
        return (C_new, n_new, m_new), h

    init = (jnp.zeros((Bsz, H, Dh, Dh), f32), jnp.zeros((Bsz, H, Dh), f32),
            jnp.zeros((Bsz, H), f32))
    _, hs = lax.scan(step, init, (qc, kc, vc, ic, lfc))
    return hs.transpose(1, 0, 3, 2, 4).reshape(Bsz, S, H, Dh)


def even_mixer(xn, w_in, w_conv, b_gate, g_head, w_out):
    Bsz, S, _ = xn.shape
    proj = xn @ w_in
    c1 = CONV_DIM
    c3 = 3 * CONV_DIM
    bg, cg, xa, q, k, v, o, gates = jnp.split(
        proj, [c1, 2 * c1, c3, c3 + MLSTM_DIM, c3 + 2 * MLSTM_DIM,
               c3 + 3 * MLSTM_DIM, c3 + 4 * MLSTM_DIM], axis=-1)
    ya = bg * causal_short_conv(cg * xa, w_conv)
    gates = gates + b_gate
    i_raw, f_raw = gates[..., :MLSTM_HEADS], gates[..., MLSTM_HEADS:]
    shp = (Bsz, S, MLSTM_HEADS, MLSTM_HEAD_DIM)
    h = mlstm_chunkwise(q.reshape(shp), k.reshape(shp), v.reshape(shp), i_raw, f_raw)
    h = h * lax.rsqrt(jnp.mean(h * h, axis=-1, keepdims=True) + RMS_EPS)
    h = h * g_head.astype(jnp.float32).reshape(MLSTM_HEADS, MLSTM_HEAD_DIM)
    yb = jax.nn.sigmoid(o) * h.reshape(Bsz, S, MLSTM_DIM).astype(xn.dtype)
    return jnp.concatenate([ya, yb], axis=-1) @ w_out


def odd_mixer(xn, w_in, g_v, w_s, b_s, w_out):
    Bsz, S, _ = xn.shape
    nc = S // SGU_CHUNK
    z = jax.nn.gelu(xn @ w_in)
    u, v = z[..., :SGU_DIM], z[..., SGU_DIM:]
    v = rmsnorm(v, g_v).reshape(Bsz, nc, SGU_CHUNK, SGU_GROUPS, SGU_GROUP_DIM)
    ws = w_s * jnp.tril(jnp.ones((SGU_CHUNK, SGU_CHUNK), dtype=w_s.dtype))
    v = jnp.einsum('gts,bcsgd->bctgd', ws, v) + b_s.T[None, None, :, :, None]
    return (u * v.reshape(Bsz, S, SGU_DIM)) @ w_out


def swiglu(xn, w_in, w_out):
    h = xn @ w_in
    g, up = h[..., :D_FF], h[..., D_FF:]
    return (jax.nn.silu(g) * up) @ w_out


def setup_inputs(seed: int = 0) -> dict:
    key = jax.random.key(seed)
    ks = jax.random.split(key, 16)
    nrm = jax.random.normal
    f32 = jnp.float32
    x = nrm(ks[0], (BATCH, SEQ, D_MODEL), f32)
    norm_mix = 1.0 + 0.02 * nrm(ks[1], (DEPTH, D_MODEL), f32)
    norm_ffn = 1.0 + 0.02 * nrm(ks[2], (DEPTH, D_MODEL), f32)
    even_w_in = nrm(ks[3], (N_EVEN, D_MODEL, EVEN_IN), f32) * D_MODEL ** -0.5
    even_w_conv = nrm(ks[4], (N_EVEN, CONV_WIDTH, CONV_DIM), f32) * CONV_WIDTH ** -0.5
    b_i = 0.1 * nrm(ks[5], (N_EVEN, MLSTM_HEADS), f32)
    b_f = 3.0 + 0.5 * nrm(jax.random.fold_in(ks[5], 1), (N_EVEN, MLSTM_HEADS), f32)
    even_b_gate = jnp.concatenate([b_i, b_f], axis=-1)
    even_g_head = 1.0 + 0.02 * nrm(ks[6], (N_EVEN, MLSTM_DIM), f32)
    even_w_out = nrm(ks[7], (N_EVEN, CONV_DIM + MLSTM_DIM, D_MODEL), f32) * (CONV_DIM + MLSTM_DIM) ** -0.5
    odd_w_in = nrm(ks[8], (N_ODD, D_MODEL, 2 * SGU_DIM), f32) * D_MODEL ** -0.5
    odd_g_v = 1.0 + 0.02 * nrm(ks[9], (N_ODD, SGU_DIM), f32)
    odd_w_s = nrm(ks[10], (N_ODD, SGU_GROUPS, SGU_CHUNK, SGU_CHUNK), f32) * SGU_CHUNK ** -0.5
    odd_b_s = 1.0 + 0.02 * nrm(ks[11], (N_ODD, SGU_GROUPS, SGU_CHUNK), f32)
    odd_w_out = nrm(ks[12], (N_ODD, SGU_DIM, D_MODEL), f32) * SGU_DIM ** -0.5
    ffn_w_in = nrm(ks[13], (DEPTH, D_MODEL, 2 * D_FF), f32) * D_MODEL ** -0.5
    ffn_w_out = nrm(ks[14], (DEPTH, D_FF, D_MODEL), f32) * D_FF ** -0.5
    norm_final = 1.0 + 0.02 * nrm(ks[15], (D_MODEL,), f32)
    return {"x": x, "norm_mix": norm_mix, "norm_ffn": norm_ffn,
            "even_w_in": even_w_in, "even_w_conv": even_w_conv, "even_b_gate": even_b_gate,
            "even_g_head": even_g_head, "even_w_out": even_w_out,
            "odd_w_in": odd_w_in, "odd_g_v": odd_g_v, "odd_w_s": odd_w_s,
            "odd_b_s": odd_b_s, "odd_w_out": odd_w_out,
            "ffn_w_in": ffn_w_in, "ffn_w_out": ffn_w_out, "norm_final": norm_final}


def reference(x, norm_mix, norm_ffn, even_w_in, even_w_conv, even_b_gate, even_g_head,
              even_w_out, odd_w_in, odd_g_v, odd_w_s, odd_b_s, odd_w_out,
              ffn_w_in, ffn_w_out, norm_final):
    for layer in range(DEPTH):
        j = layer // 2
        xn = rmsnorm(x, norm_mix[layer])
        if layer % 2 == 0:
            x = x + even_mixer(xn, even_w_in[j], even_w_conv[j], even_b_gate[j],
                               even_g_head[j], even_w_out[j])
        else:
            x = x + odd_mixer(xn, odd_w_in[j], odd_g_v[j], odd_w_s[j], odd_b_s[j], odd_w_out[j])
        x = x + swiglu(rmsnorm(x, norm_ffn[layer]), ffn_w_in[layer], ffn_w_out[layer])
    return rmsnorm(x, norm_final)
```

```python
from contextlib import ExitStack
from concourse.bass_utils import run_bass_kernel_spmd
import numpy as np
import concourse.bass as bass
import concourse.mybir as mybir

F32 = mybir.dt.float32
BF16 = mybir.dt.bfloat16
ALU = mybir.AluOpType
AF = mybir.ActivationFunctionType
AX = mybir.AxisListType


class _Op:
    __slots__ = ("eng", "idx", "fn", "waits", "signal", "dma_sem", "dma_cnt", "sigval", "isdma")

    def __init__(self, eng, idx, fn):
        self.eng = eng
        self.idx = idx
        self.fn = fn
        self.waits = []
        self.signal = False
        self.dma_sem = None
        self.dma_cnt = 0
        self.sigval = 0
        self.isdma = False


def _ap_range(ap):
    sp = str(ap.space)
    name = ap.tensor.name
    if "DRAM" in sp.upper() or sp.upper() not in ("SB", "PSUM"):
        return ("D:" + name, 0, 1)
    esz = mybir.dt.size(ap.dtype)
    pat = ap.ap
    pstride = pat[0][0]
    off = ap.offset % pstride if pstride > 0 else ap.offset
    ext = 0
    for st, cnt in pat[1:]:
        ext += abs(st) * (cnt - 1)
    lo = off * esz
    hi = (off + ext + 1) * esz
    if sp.upper() == "PSUM":
        lo, hi = (lo // 2048) * 2048, ((hi + 2047) // 2048) * 2048
    return (sp + ":" + name, lo, hi)


class Prog:
    ENGS = ("pe", "act", "dve", "pool", "sp")

    def __init__(self, nc):
        self.nc = nc
        self.ops = {e: [] for e in self.ENGS}
        self.segs = {}
        self.seen = {e: {f: -1 for f in self.ENGS} for e in self.ENGS}
        self.seen_sem = {e: {} for e in self.ENGS}
        self.dma_sems = {}
        self.extra_sems = []

    def _touch(self, key, lo, hi, op, is_write, deps):
        segs = self.segs.setdefault(key, [])
        new = []
        for s in segs:
            if s[1] <= lo or s[0] >= hi:
                new.append(s)
                continue
            cuts = [s[0]] + [c for c in (lo, hi) if s[0] < c < s[1]] + [s[1]]
            for a, b in zip(cuts[:-1], cuts[1:]):
                new.append([a, b, s[2], dict(s[3])])
        new.sort(key=lambda s: s[0])
        out = []
        cur = lo
        for s in new:
            if s[1] <= lo or s[0] >= hi:
                out.append(s)
                continue
            if s[0] > cur:
                out.append([cur, s[0], None, {}])
            out.append(s)
            cur = s[1]
        if cur < hi:
            out.append([cur, hi, None, {}])
        out.sort(key=lambda s: s[0])
        for s in out:
            if s[1] <= lo or s[0] >= hi:
                continue
            if s[2] is not None:
                deps.append((s[2], "raw" if not is_write else "waw"))
            if is_write:
                for r in s[3].values():
                    deps.append((r, "war"))
                s[2] = op
                s[3] = {}
            else:
                s[3][op.eng if op.dma_sem is None and not op.isdma else ("dma", id(op))] = op
        merged = []
        for s in out:
            if merged and merged[-1][1] == s[0] and merged[-1][2] is s[2] and merged[-1][3] == s[3]:
                merged[-1][1] = s[1]
            else:
                merged.append(s)
        self.segs[key] = merged

    def add(self, eng, fn, reads=(), writes=(), dma_slot=None, n_dma=1):
        op = _Op(eng, len(self.ops[eng]), fn)
        op.isdma = dma_slot is not None
        deps = []
        for ap in reads:
            k, lo, hi = ap if isinstance(ap, tuple) else _ap_range(ap)
            self._touch(k, lo, hi, op, False, deps)
        for ap in writes:
            k, lo, hi = ap if isinstance(ap, tuple) else _ap_range(ap)
            self._touch(k, lo, hi, op, True, deps)
        if dma_slot is not None:
            self.dma_sems[dma_slot] = self.dma_sems.get(dma_slot, 0) + 16 * n_dma
            op.dma_sem = dma_slot
            op.dma_cnt = self.dma_sems[dma_slot]
        best = {}
        for dop, kind in deps:
            if dop is op:
                continue
            if dop.dma_sem is not None:
                cur = self.seen_sem[eng].get(dop.dma_sem, 0)
                if dop.dma_cnt > cur:
                    self.seen_sem[eng][dop.dma_sem] = dop.dma_cnt
                    op.waits.append(("sem", dop.dma_sem, dop.dma_cnt))
                continue
            if dop.eng == eng and not op.isdma:
                if kind != "raw" or eng == "pe":
                    continue
            if dop.idx > best.get(dop.eng, -1):
                best[dop.eng] = dop.idx
        for f, idx in best.items():
            if idx > self.seen[eng][f]:
                self.seen[eng][f] = idx
                self.ops[f][idx].signal = True
                op.waits.append(("eng", f, idx))
        self.ops[eng].append(op)
        return op

    def emit(self, block, sems_eng, sems_dma, extra=None):
        nc = self.nc
        for e in self.ENGS:
            c = 0
            for op in self.ops[e]:
                if op.signal:
                    c += 1
                    op.sigval = c
        handles = {"pe": nc.tensor, "act": nc.scalar, "dve": nc.vector, "pool": nc.gpsimd, "sp": nc.sync}

        def body(e):
            def f(h):
                for op in self.ops[e]:
                    for w in op.waits:
                        if w[0] == "sem":
                            h.wait_ge(sems_dma[w[1]], w[2])
                        else:
                            h.wait_ge(sems_eng[w[1]], self.ops[w[1]][w[2]].sigval)
                    ins = op.fn(h)
                    if op.dma_sem is not None:
                        pass
                    elif op.signal:
                        ins.then_inc(sems_eng[e], 1)
                if extra and e in extra:
                    extra[e](h)
            return f
        block.tensor(body("pe"))
        block.scalar(body("act"))
        block.vector(body("dve"))
        block.gpsimd(body("pool"))
        block.sync(body("sp"))


NT = 16
TOK = 2048
D = 1024
EPS = 1e-6
KSC = 128 ** -0.5
SUMW = 532

O_X = 0
O_XNT = 65536
O_CST = 98304
O_GB = O_CST + 8192
O_XS = O_GB + 8192
O_PH = O_XS + 4096
PH_SIZE = 90112
ARENA = O_PH + PH_SIZE


class K:
    pass


def build_program(stages, final_norm=True):
    nc = bass.Bass("TRN2", target_bir_lowering=False)
    P = Prog(nc)
    dt_in = {}

    def din(name, shape):
        t = nc.dram_tensor(name, list(shape), F32, kind="ExternalInput")
        dt_in[name] = t
        return t.ap()

    x_d = din("x", [TOK, D])
    cf_d = din("c_f32", [128, 384])
    cb_d = din("c_bf", [128, 640])
    fl_d = din("flags", [128, 12])
    gmix_d = din("gmix_b", [4, 128, D])
    gffn_d = din("gffn_b", [4, 128, D])
    gfin_d = din("gfin_b", [128, D])
    bgate_d = din("bgate_b", [2, 128, 8])
    wconv_d = din("wconvT", [2, 128, 12])
    ghead_d = din("ghead_b", [2, 128, 512])
    gv_d = din("gv_b", [2, 128, D])
    bs_d = din("bs_b", [2, 128, D])
    ewin_d = din("even_w_in", [2, 1024, 3592])
    ewout_d = din("even_w_out", [2, 1024, 1024])
    owin_d = din("odd_w_in", [2, 1024, 2048])
    ows_d = din("odd_w_s", [2, 8, 128, 128])
    owout_d = din("odd_w_out", [2, 1024, 1024])
    fwin_d = din("ffn_w_in", [4, 1024, 5632])
    fwout_d = din("ffn_w_out", [4, 2816, 1024])
    y_d = nc.dram_tensor("y", [TOK, D], F32, kind="ExternalOutput").ap()
    ccin = [nc.dram_tensor(f"ccin{i}", [128, SUMW], F32) for i in range(2)]
    ccout = [nc.dram_tensor(f"ccout{i}", [512, SUMW], F32) for i in range(2)]

    es = ExitStack()
    A = es.enter_context(nc.sbuf_tensor("arena", [128, ARENA // 4], F32))
    PS = [es.enter_context(nc.psum_tensor(f"ps{i}", [128, 512], F32)) for i in range(8)]
    sems_eng = {e: es.enter_context(nc.semaphore("s_" + e)) for e in Prog.ENGS}
    sems_dma = {}

    def dsem(name):
        if name not in sems_dma:
            sems_dma[name] = es.enter_context(nc.semaphore("d_" + name))
        return sems_dma[name]

    def f32v(off, n, parts=128):
        return A[0:parts, off // 4: off // 4 + n]

    def bfv(off, n, parts=128):
        return A[0:parts, off // 4: off // 4 + (n + 1) // 2].bitcast(BF16)[:, 0:n]

    X = f32v(O_X, NT * D).rearrange("p (t d) -> p t d", d=D)
    XNT = bfv(O_XNT, 8 * TOK).rearrange("p (k t) -> p k t", t=TOK)
    c = O_CST
    IDF = f32v(c, 128); c += 512
    MST = f32v(c, 128); c += 512
    ONES = f32v(c, 128); c += 512
    IDB = bfv(c, 128); c += 256
    MNEG = bfv(c, 512); c += 1024
    FLG = f32v(c, 12); c += 48
    SSQ = f32v(c, 16); c += 64
    RSTD = f32v(c, 16); c += 64
    BGATE = f32v(c, 8); c += 32
    WCONV = f32v(c, 12); c += 48
    SM = f32v(c, 64); c += 256
    ZER = f32v(c, 128); c += 512
    GHEAD = f32v(c, 512); c += 2048
    assert c <= O_CST + 8192, c
    GB = [f32v(O_GB + i * 4096, D) for i in range(2)]
    XS = [bfv(O_XS + i * 2048, D) for i in range(2)]

    def mm(out, lhsT, rhs, start=True, stop=True):
        P.add("pe", lambda h, o=out, l=lhsT, r=rhs, s=start, e=stop: h.matmul(o, l, r, start=s, stop=e),
              reads=[lhsT, rhs], writes=[out])

    def tr(out, in_, ident):
        P.add("pe", lambda h, o=out, i=in_, d=ident: h.transpose(o, i, d), reads=[in_, ident], writes=[out])

    def act(out, in_, func, bias=None, scale=None, accum=None):
        rd = [in_] + [v for v in (bias, scale) if v is not None and not isinstance(v, (int, float))]
        wr = [out] + ([accum] if accum is not None else [])
        kw = {}
        if bias is not None:
            kw["bias"] = bias
        if scale is not None:
            kw["scale"] = scale
        if accum is not None:
            kw["accum_out"] = accum
        P.add("act", lambda h, o=out, i=in_, f=func, k=kw: h.activation(out=o, in_=i, func=f, **k), reads=rd, writes=wr)

    def tt(out, in0, in1, op, eng="dve"):
        P.add(eng, lambda h, o=out, a=in0, b=in1, p=op: h.tensor_tensor(out=o, in0=a, in1=b, op=p),
              reads=[in0, in1], writes=[out])

    def ts(out, in0, s1, s2, op0, op1=None, eng="dve"):
        rd = [in0] + [v for v in (s1, s2) if v is not None and not isinstance(v, (int, float))]
        if op1 is None:
            P.add(eng, lambda h, o=out, a=in0, x=s1, p=op0: h.tensor_scalar(out=o, in0=a, scalar1=x, scalar2=None, op0=p),
                  reads=rd, writes=[out])
        else:
            P.add(eng, lambda h, o=out, a=in0, x=s1, y=s2, p=op0, q=op1: h.tensor_scalar(out=o, in0=a, scalar1=x, scalar2=y, op0=p, op1=q),
                  reads=rd, writes=[out])

    def stt(out, in0, scalar, in1, op0, op1, accum=None):
        rd = [in0, in1] + ([scalar] if not isinstance(scalar, (int, float)) else [])
        wr = [out] + ([accum] if accum is not None else [])
        if accum is None:
            P.add("dve", lambda h, o=out, a=in0, s=scalar, b=in1, p=op0, q=op1: h.scalar_tensor_tensor(out=o, in0=a, scalar=s, in1=b, op0=p, op1=q),
                  reads=rd, writes=wr)
        else:
            P.add("dve", lambda h, o=out, a=in0, s=scalar, b=in1, p=op0, q=op1, ac=accum: h.scalar_tensor_tensor(out=o, in0=a, scalar=s, in1=b, op0=p, op1=q, accum_out=ac),
                  reads=rd, writes=wr)

    def cp(out, in_, eng="dve"):
        if eng == "act":
            act(out, in_, AF.Copy)
        else:
            P.add(eng, lambda h, o=out, i=in_: h.tensor_copy(out=o, in_=i), reads=[in_], writes=[out])

    def scan(out, d0, d1, init, op0, op1):
        rd = [d0, d1] + ([init] if not isinstance(init, (int, float)) else [])
        P.add("dve", lambda h, o=out, a=d0, b=d1, i=init, p=op0, q=op1: h.tensor_tensor_scan(out=o, data0=a, data1=b, initial=i, op0=p, op1=q),
              reads=rd, writes=[out])

    def red(out, in_, op):
        P.add("dve", lambda h, o=out, i=in_, p=op: h.tensor_reduce(out=o, in_=i, axis=AX.X, op=p), reads=[in_], writes=[out])

    def recip(out, in_):
        P.add("dve", lambda h, o=out, i=in_: h.reciprocal(out=o, in_=i), reads=[in_], writes=[out])

    def memset(out, val, eng="pool"):
        P.add(eng, lambda h, o=out, v=val: h.memset(o, v), writes=[out])

    def dma(queue, slot, pairs, slot_ap=None, extra_reads=()):
        sem = dsem(slot)
        def fn(h, pairs=pairs, sem=sem):
            r = None
            for o, i in pairs:
                r = h.dma_start(out=o, in_=i).then_inc(sem, 16)
            return r
        wr = [slot_ap] if slot_ap is not None else [o for o, _ in pairs]
        P.add(queue, fn, reads=[i for _, i in pairs] + list(extra_reads), writes=wr, dma_slot=slot, n_dma=len(pairs))

    _bank = [0]

    def bank():
        b = PS[_bank[0] % 8]
        _bank[0] += 1
        return b

    dma("sp", "cst", [(A[:, O_CST // 4: O_CST // 4 + 384], cf_d), (FLG, fl_d)], slot_ap=A[:, O_CST // 4: (O_CST + 8192) // 4])
    dma("pool", "cstb", [(bfv(O_CST + 1536, 640), cb_d)], slot_ap=bfv(O_CST + 1536, 640))
    memset(ZER, 0.0)
    xin = x_d.rearrange("(t p) d -> p t d", p=128)
    for q4 in range(4):
        dma("sp", f"xin{q4}", [(X[:, q4 * 4:(q4 + 1) * 4, :], xin[:, q4 * 4:(q4 + 1) * 4, :])])

    def norm_to_xnt(g_src, gslot):
        g_b = GB[gslot]
        dma("sp", f"gb{gslot}", [(g_b, g_src)])
        for t in range(NT):
            act(XS[t % 2], X[:, t, :], AF.Square, accum=SSQ[:, t:t + 1])
        ts(RSTD, SSQ, 1.0 / D, EPS, ALU.mult, ALU.add)
        act(RSTD, RSTD, AF.Sqrt)
        recip(RSTD, RSTD)
        for t in range(NT):
            xs = XS[t % 2]
            stt(xs, X[:, t, :], RSTD[:, t:t + 1], g_b, ALU.mult, ALU.mult)
            pb = bank()
            pbb = pb[:, :].bitcast(BF16)
            for k in range(8):
                tr(pbb[:, k * 128:(k + 1) * 128], xs[:, k * 128:(k + 1) * 128], IDB)
            cp(XNT[:, :, t * 128:(t + 1) * 128], pbb.rearrange("p (k t) -> p k t", t=128), eng="act")

    def resid_add(t, half, ps):
        xv = X[:, t, half * 512:(half + 1) * 512]
        tt(xv, ps, xv, ALU.add)

    def ffn(l):
        groups = [(0, 6), (6, 6), (12, 5), (17, 5)]
        WI = [bfv(O_PH + s * 36864, 8 * 2 * 768).rearrange("p (k u n) -> p k u n", u=2, n=768) for s in range(2)]
        WO = [bfv(O_PH + s * 36864 + 24576, 6 * 1024).rearrange("p (j d) -> p j d", d=1024) for s in range(2)]
        ACTB = [bfv(O_PH + 73728 + s * 6144, 6 * 512).rearrange("p (j t) -> p j t", t=512) for s in range(2)]
        SG = [f32v(O_PH + 86016 + s * 2048, 512) for s in range(2)]
        it = 0
        for gi, (c0, n) in enumerate(groups):
            s = gi % 2
            slot_ap = bfv(O_PH + s * 36864, 18432)
            win = fwin_d[l].rearrange("(k p) n -> p k n", p=128)
            dma("pool", f"ffw{s}", [
                (WI[s][:, :, 0, 0:n * 128], win[:, :, c0 * 128:(c0 + n) * 128]),
                (WI[s][:, :, 1, 0:n * 128], win[:, :, 2816 + c0 * 128:2816 + (c0 + n) * 128]),
                (WO[s][:, 0:n, :], fwout_d[l][c0 * 128:(c0 + n) * 128, :].rearrange("(j p) d -> p j d", p=128)),
            ], slot_ap=slot_ap)
            for b in range(4):
                ab = ACTB[it % 2]
                it += 1
                for j in range(n):
                    pg = bank()
                    pu = bank()
                    for k in range(8):
                        mm(pg[:, :], WI[s][:, k, 0, j * 128:(j + 1) * 128], XNT[:, k, b * 512:(b + 1) * 512], k == 0, k == 7)
                    for k in range(8):
                        mm(pu[:, :], WI[s][:, k, 1, j * 128:(j + 1) * 128], XNT[:, k, b * 512:(b + 1) * 512], k == 0, k == 7)
                    sg = SG[j % 2]
                    act(sg, pg[:, :], AF.Silu)
                    tt(ab[:, j, :], pu[:, :], sg, ALU.mult)
                for i in range(4):
                    t = b * 4 + i
                    for half in range(2):
                        po = bank()
                        for j in range(n):
                            mm(po[:, :], ab[:, j, i * 128:(i + 1) * 128], WO[s][:, j, half * 512:(half + 1) * 512], j == 0, j == n - 1)
                        resid_add(t, half, po[:, :])

    def odd_mixer(j):
        o = O_PH
        WU = bfv(o, 8 * 1024).rearrange("p (k n) -> p k n", n=1024); o += 16384
        WV = bfv(o, 8 * 1024).rearrange("p (k n) -> p k n", n=1024); o += 16384
        WOUT = bfv(o, 8 * 1024).rearrange("p (k n) -> p k n", n=1024); o += 16384
        WST = bfv(o, 8 * 128).rearrange("p (g t) -> p g t", t=128); o += 2048
        BSB = f32v(o, 1024); o += 4096
        GVB = f32v(o, 1024); o += 4096
        UT = bfv(o, 8 * 512).rearrange("p (g t) -> p g t", t=512); o += 8192
        VG = f32v(o, 1024); o += 4096
        VN = bfv(o, 1024); o += 2048
        T1 = f32v(o, 1024); o += 4096
        GT = bfv(o, 1024).rearrange("p (g t) -> p g t", t=128); o += 2048
        WSS = f32v(o, 1024).rearrange("p (g s) -> p g s", s=128); o += 4096
        JNK = bfv(o, 1024); o += 2048
        assert o <= O_PH + PH_SIZE
        win = owin_d[j].rearrange("(k p) n -> p k n", p=128)
        dma("pool", "owu", [(WU, win[:, :, 0:1024])])
        dma("pool", "owv", [(WV, win[:, :, 1024:2048])])
        dma("pool", "owo", [(WOUT, owout_d[j].rearrange("(k p) n -> p k n", p=128))])
        dma("sp", "ows", [(WSS, ows_d[j].rearrange("g t s -> t g s"))])
        dma("sp", "obs", [(BSB, bs_d[j])])
        dma("sp", "ogv", [(GVB, gv_d[j])])
        for g in range(8):
            pb = bank()
            tr(pb[:, 0:128], WSS[:, g, :], IDF)
            tt(WST[:, g, :], pb[:, 0:128], MST, ALU.mult)
        for b in range(4):
            for fc in range(8):
                pb = bank()
                for k in range(8):
                    mm(pb[:, :], WU[:, k, fc * 128:(fc + 1) * 128], XNT[:, k, b * 512:(b + 1) * 512], k == 0, k == 7)
                act(UT[:, fc, :], pb[:, :], AF.Gelu_apprx_tanh)
            for i in range(4):
                t = b * 4 + i
                pv = [bank(), bank()]
                for half in range(2):
                    for k in range(8):
                        mm(pv[half][:, :], XNT[:, k, t * 128:(t + 1) * 128], WV[:, k, half * 512:(half + 1) * 512], k == 0, k == 7)
                for half in range(2):
                    act(VG[:, half * 512:(half + 1) * 512], pv[half][:, :], AF.Gelu_apprx_tanh)
                ss = SM[:, 0:1]
                stt(JNK, VG, 1.0, VG, ALU.mult, ALU.mult, accum=ss)
                rs = SM[:, 1:2]
                ts(rs, ss, 1.0 / 1024, EPS, ALU.mult, ALU.add)
                act(rs, rs, AF.Sqrt)
                recip(rs, rs)
                stt(VN, VG, rs, GVB, ALU.mult, ALU.mult)
                pg = [bank(), bank()]
                for g in range(8):
                    mm(pg[g // 4][:, (g % 4) * 128:(g % 4 + 1) * 128], VN[:, g * 128:(g + 1) * 128], WST[:, g, :])
                for hf in range(2):
                    tt(T1[:, hf * 512:(hf + 1) * 512], pg[hf][:, :], BSB[:, hf * 512:(hf + 1) * 512], ALU.add)
                tt(GT, T1.rearrange("p (g t) -> p g t", t=128), UT[:, :, i * 128:(i + 1) * 128], ALU.mult)
                for half in range(2):
                    po = bank()
                    for g in range(8):
                        mm(po[:, :], GT[:, g, :], WOUT[:, g, half * 512:(half + 1) * 512], g == 0, g == 7)
                    resid_add(t, half, po[:, :])

    K.norm_to_xnt = norm_to_xnt
    K.ffn = ffn
    K.odd_mixer = odd_mixer
    class Bump:
        def __init__(self, base, limit):
            self.base, self.limit, self.cur = base, limit, base

        def reset(self):
            self.cur = self.base

        def _take(self, nbytes):
            o = self.cur
            self.cur += (nbytes + 31) // 32 * 32
            assert self.cur <= self.limit, (self.cur, self.limit)
            return o

        def f32(self, n, parts=128):
            return f32v(self._take(n * 4), n, parts)

        def bf(self, n, parts=128):
            return bfv(self._take(n * 2), n, parts)

    K.reserved = set()
    _obank = bank

    def bank():
        while True:
            i = _bank[0] % 8
            _bank[0] += 1
            if i not in K.reserved:
                return PS[i]

    def even_mixer(l, j):
        cci = j
        ws_off = [O_PH + i * 8192 for i in range(5)]
        WS = [bfv(ws_off[i], 8 * 512).rearrange("p (k n) -> p k n", n=512) for i in range(5)]
        WOUT = bfv(O_PH + 40960, 8 * 1024).rearrange("p (k n) -> p k n", n=1024)
        sm = Bump(O_PH + 57344, O_PH + 71680)
        tp = Bump(O_PH + 71680, O_PH + PH_SIZE)
        s2 = Bump(ws_off[2], ws_off[2] + 8192)
        win = ewin_d[j].rearrange("(k p) n -> p k n", p=128)

        def loadw(slot, col0):
            dma("pool", f"ew{slot}", [(WS[slot], win[:, :, col0:col0 + 512])])
        loadw(0, 0); loadw(1, 512); loadw(2, 1024)
        dma("pool", "ewo", [(WOUT, ewout_d[j].rearrange("(k p) n -> p k n", p=128))])
        loadw(3, 2048); loadw(4, 2560)
        WG = sm.bf(64).rearrange("p (k n) -> p k n", n=8)
        dma("pool", "ewg", [(WG, win[:, :, 3584:3592])])
        dma("sp", "ebg", [(BGATE, bgate_d[j])])
        dma("sp", "ewc", [(WCONV, wconv_d[j])])
        dma("sp", "egh", [(GHEAD, ghead_d[j])])
        v3 = lambda ap: ap.rearrange("p (c h) -> p c h", h=4)
        ZH = sm.f32(8).rearrange("p (c t) -> p c t", t=2)
        Z01 = sm.f32(8).rearrange("p (c t) -> p c t", t=2)
        BG01 = sm.f32(8).rearrange("p (c t) -> p c t", t=2)
        SUMM = sm.f32(SUMW)
        WC = WCONV.rearrange("p (c j) -> p c j", j=3)
        tp.reset()
        YAB = tp.bf(4 * 512).rearrange("p (c t) -> p c t", t=512)
        XA = tp.f32(512)
        Z = [tp.f32(514) for _ in range(2)]
        ACC = tp.f32(512)
        memset(ZH, 0.0)
        it = 0
        for b in range(4):
            for cc in range(4):
                pbg, pcg, pxa = bank(), bank(), bank()
                for pp, sl in ((pbg, 0), (pcg, 1), (pxa, 2)):
                    for k in range(8):
                        mm(pp[:, :], WS[sl][:, k, cc * 128:(cc + 1) * 128], XNT[:, k, b * 512:(b + 1) * 512], k == 0, k == 7)
                z = Z[it % 2]
                it += 1
                cp(XA, pxa[:, :], eng="act")
                cp(z[:, 0:2], ZH[:, cc, :], eng="pool")
                tt(z[:, 2:514], pcg[:, :], XA, ALU.mult)
                ts(ACC, z[:, 2:514], WC[:, cc, 2:3], None, ALU.mult)
                stt(ACC, z[:, 1:513], WC[:, cc, 1:2], ACC, ALU.mult, ALU.add)
                stt(ACC, z[:, 0:512], WC[:, cc, 0:1], ACC, ALU.mult, ALU.add)
                tt(YAB[:, cc, :], pbg[:, :], ACC, ALU.mult)
                cp(ZH[:, cc, :], z[:, 512:514], eng="pool")
                if b == 0:
                    cp(Z01[:, cc, :], z[:, 2:4], eng="pool")
                    cp(BG01[:, cc, :], pbg[:, 0:2], eng="act")
                if b == 3:
                    cp(SUMM[:, 524 + cc * 2:526 + cc * 2], z[:, 512:514], eng="pool")
            for i in range(4):
                t = b * 4 + i
                for half in range(2):
                    po = bank()
                    for cc in range(4):
                        mm(po[:, :], YAB[:, cc, i * 128:(i + 1) * 128], WOUT[:, cc, half * 512:(half + 1) * 512], cc == 0, cc == 3)
                    resid_add(t, half, po[:, :])
        loadw(0, 1536)
        loadw(1, 3072)
        GC = sm.f32(128).rearrange("p (t n) -> p t n", n=8)
        pgt = bank()
        for t in range(16):
            for k in range(8):
                mm(pgt[:, t * 8:(t + 1) * 8], XNT[:, k, t * 128:(t + 1) * 128], WG[:, k, :], k == 0, k == 7)
        tt(GC, pgt[:, 0:128].rearrange("p (t n) -> p t n", n=8), BGATE.unsqueeze(1).to_broadcast([128, 16, 8]), ALU.add)
        IC = sm.f32(64); LFC = sm.f32(64); EX = sm.f32(64)
        cp(v3(IC), GC[:, :, 0:4])
        act(v3(EX), GC[:, :, 4:8], AF.Exp, scale=-1.0)
        act(EX, EX, AF.Ln, bias=1.0)
        ts(LFC, EX, -1.0, None, ALU.mult)
        pr = bank()
        tr(pr[0:64, 0:128], IC, IDF)
        tr(pr[0:64, 128:256], LFC, IDF)
        IR = sm.f32(128, 64); LFR = sm.f32(128, 64); BR = sm.f32(128, 64); AR = sm.f32(128, 64)
        CMR = sm.f32(128, 64); NEGMR = sm.f32(128, 64)
        cp(IR, pr[0:64, 0:128], eng="act")
        cp(LFR, pr[0:64, 128:256], eng="act")
        scan(BR, LFR, ZER[0:64, :], 0.0, ALU.add, ALU.add)
        tt(AR, IR, BR, ALU.subtract)
        scan(CMR, AR, AR, -1e30, ALU.max, ALU.max)
        pc = bank()
        tr(pc[:, 0:64], AR, IDF[0:64, 0:64])
        tr(pc[:, 64:128], BR, IDF[0:64, 0:64])
        tr(pc[:, 128:192], CMR, IDF[0:64, 0:64])
        ABC = sm.f32(192)
        cp(ABC, pc[:, 0:192], eng="act")
        AC, BC, CMC = ABC[:, 0:64], ABC[:, 64:128], ABC[:, 128:192]
        TB = IR
        TB2 = LFR
        cp(TB, BR[:, 127:128].to_broadcast([64, 128]))
        cp(TB2, CMR[:, 127:128].to_broadcast([64, 128]))
        prp = bank()
        mm(prp[:, 0:64], TB, IDF[0:64, 0:64])
        mm(prp[:, 64:128], TB2, IDF[0:64, 0:64])
        REP = sm.f32(128)
        cp(REP, prp[:, 0:128], eng="act")
        BL, AMX = REP[:, 0:64], REP[:, 64:128]
        BINC = sm.f32(64)
        for h in range(4):
            scan(v3(BINC)[:, :, h], v3(BL)[:, :, h], ZER[:, 0:16], 0.0, ALU.add, ALU.add)
        REM = sm.f32(64)
        tt(REM, BINC, BL, ALU.subtract)
        tt(v3(REM), v3(BINC)[:, 15:16, :].to_broadcast([128, 16, 4]), v3(REM), ALU.subtract)
        VAL = sm.f32(64)
        tt(VAL, AMX, REM, ALU.add)
        MLOC = sm.f32(4)
        red(MLOC, VAL.rearrange("p (c h) -> p h c", h=4), ALU.max)
        TT_ = sm.f32(64)
        tt(v3(TT_), v3(REM), MLOC.unsqueeze(1).to_broadcast([128, 16, 4]), ALU.subtract)
        WEND = sm.f32(64)
        tt(WEND, AC, TT_, ALU.add)
        act(WEND, WEND, AF.Exp)
        tp.reset()
        WKA = [tp.bf(512).rearrange("p (h d) -> p h d", d=128) for _ in range(2)]
        VEX = [tp.bf(520).rearrange("p (h e) -> p h e", e=130) for _ in range(2)]
        for vv in VEX:
            memset(vv[:, :, 128:130], 1.0)
        K.reserved = {4, 5, 6, 7}
        CA = [PS[4 + h] for h in range(4)]
        for c in range(16):
            tsl = slice(c * 128, (c + 1) * 128)
            pk, pv_ = bank(), bank()
            for k in range(8):
                mm(pk[:, :], XNT[:, k, tsl], WS[3][:, k, :], k == 0, k == 7)
            for k in range(8):
                mm(pv_[:, :], XNT[:, k, tsl], WS[4][:, k, :], k == 0, k == 7)
            wk = WKA[c % 2]
            vx = VEX[c % 2]
            stt(wk, pk[:, :].rearrange("p (h d) -> p h d", d=128), KSC,
                v3(WEND)[:, c, :].unsqueeze(2).to_broadcast([128, 4, 128]), ALU.mult, ALU.mult)
            cp(vx[:, :, 0:128], pv_[:, :].rearrange("p (h e) -> p h e", e=128), eng="act")
            for h in range(4):
                mm(CA[h][:, 0:129], wk[:, h, :], vx[:, h, 0:129], c == 0, c == 15)
        for h in range(4):
            cp(SUMM[:, h * 129:(h + 1) * 129], CA[h][:, 0:129], eng="act")
        K.reserved = set()
        cp(SUMM[:, 516:520], MLOC)
        cp(SUMM[:, 520:524], v3(BINC)[:, 15, :])
        dma("sp", f"ccin{cci}", [(ccin[cci].ap(), SUMM)])
        csem = dsem(f"cc{cci}")

        def ccfn(h, cci=cci, csem=csem):
            return h.collective_compute("AllGather", ALU.bypass, replica_groups=[[0, 1, 2, 3], [4, 5, 6, 7]],
                                        ins=[ccin[cci].ap().opt()], outs=[ccout[cci].ap().opt()]).then_inc(csem)
        _cop = P.add("pool", ccfn, reads=[ccin[cci].ap()], writes=[ccout[cci].ap()], dma_slot=f"cc{cci}", n_dma=1)
        _cop.dma_cnt = 1
        P.dma_sems[f"cc{cci}"] = 1
        s2.reset()
        G = s2.f32(SUMW)
        CST_ = s2.f32(516).rearrange("p (h e) -> p h e", e=129)
        CBF = s2.bf(520).rearrange("p (h e) -> p h e", e=130)
        TMPC = s2.f32(129)
        MREP = sm.f32(4); HZ = sm.f32(8)
        BJ = sm.f32(4); MJ = sm.f32(4); T1_ = sm.f32(4); MN = sm.f32(4); S12 = sm.f32(8)
        memset(CST_, 0.0)
        memset(MREP, 0.0)
        memset(HZ, 0.0)
        for jr in range(3):
            dma("sp", "gload", [(G, ccout[cci].ap()[jr * 128:(jr + 1) * 128, :])])
            inc = FLG[:, jr:jr + 1]; neg = FLG[:, 4 + jr:5 + jr]; prv = FLG[:, 8 + jr:9 + jr]
            ts(BJ, G[:, 520:524], inc, None, ALU.mult)
            ts(MJ, G[:, 516:520], inc, neg, ALU.mult, ALU.add)
            tt(T1_, MREP, BJ, ALU.add)
            tt(MN, T1_, MJ, ALU.max)
            tt(S12[:, 0:4], T1_, MN, ALU.subtract)
            tt(S12[:, 4:8], MJ, MN, ALU.subtract)
            act(S12, S12, AF.Exp)
            for h in range(4):
                ts(TMPC, G[:, h * 129:(h + 1) * 129], S12[:, 4 + h:5 + h], None, ALU.mult)
                stt(CST_[:, h, :], CST_[:, h, :], S12[:, h:h + 1], TMPC, ALU.mult, ALU.add)
            cp(MREP, MN)
            stt(HZ, G[:, 524:532], prv, HZ, ALU.mult, ALU.add)
        cp(CBF[:, :, 0:129], CST_, eng="act")
        tp.reset()
        QT = tp.bf(512).rearrange("p (h d) -> p h d", d=128)
        KT = tp.bf(512).rearrange("p (h d) -> p h d", d=128)
        WK = tp.bf(512).rearrange("p (h d) -> p h d", d=128)
        VX = tp.bf(520).rearrange("p (h e) -> p h e", e=130)
        OG = tp.bf(512)
        RC = tp.f32(512, 64)
        DD = tp.bf(512).rearrange("p (h d) -> p h d", d=128)
        PP = tp.bf(512).rearrange("p (h d) -> p h d", d=128)
        QCS = tp.f32(516)
        ND = tp.f32(516)
        JNK = tp.bf(128)
        YB = tp.bf(512)
        YBT = tp.bf(512).rearrange("p (h d) -> p h d", d=128)
        DY = tp.bf(512).rearrange("p (c t) -> p c t", t=128)
        DEN = tp.f32(4); NDEN = tp.f32(4); RDEN = tp.f32(4); SSH = tp.f32(4); RSH = tp.f32(4)
        D0 = tp.f32(4); D1 = tp.f32(4); D2 = tp.f32(4)
        HZ3 = HZ.rearrange("p (c t) -> p c t", t=2)
        tt(D0, HZ3[:, :, 0], WC[:, :, 0], ALU.mult)
        tt(D2, HZ3[:, :, 1], WC[:, :, 1], ALU.mult)
        tt(D0, D0, D2, ALU.add)
        tt(D0, D0, BG01[:, :, 0], ALU.mult)
        tt(D1, HZ3[:, :, 1], WC[:, :, 0], ALU.mult)
        tt(D1, D1, BG01[:, :, 1], ALU.mult)
        memset(DY, 0.0)
        cp(DY[:, :, 0], D0)
        cp(DY[:, :, 1], D1)
        for half in range(2):
            po = bank()
            for cc in range(4):
                mm(po[:, :], DY[:, cc, :], WOUT[:, cc, half * 512:(half + 1) * 512], cc == 0, cc == 3)
            resid_add(0, half, po[:, :])
        MNEXT = sm.f32(64); MCUR = sm.f32(64); MX = sm.f32(64); DEC = sm.f32(64); WKB = sm.f32(64)
        MCOL = sm.f32(64); INTER = sm.f32(64); FL = sm.f32(64)
        for h in range(4):
            scan(v3(MNEXT)[:, :, h], v3(AMX)[:, :, h], v3(BL)[:, :, h], MREP[:, h:h + 1], ALU.max, ALU.add)
        cp(v3(MCUR)[:, 0, :], MREP)
        cp(v3(MCUR)[:, 1:16, :], v3(MNEXT)[:, 0:15, :])
        tt(MX, MCUR, AMX, ALU.max)
        tt(DEC, MCUR, MX, ALU.subtract)
        act(DEC, DEC, AF.Exp)
        tt(WKB, AC, MX, ALU.subtract)
        act(WKB, WKB, AF.Exp)
        tt(MCOL, CMC, MCUR, ALU.max)
        tt(INTER, MCUR, MCOL, ALU.subtract)
        act(INTER, INTER, AF.Exp)
        tt(FL, BC, MCOL, ALU.add)
        act(FL, FL, AF.Exp, scale=-1.0)
        T64 = AR[:, 0:64]
        MR = sm.f32(1, 64)
        tt(T64, MCUR[0:64, :], IDF[0:64, 0:64], ALU.mult)
        red(MR, T64, ALU.add)
        ts(NEGMR, CMR, MR, -1.0, ALU.max, ALU.mult)
        memset(VX[:, :, 128:130], 1.0)
        ND3 = ND.rearrange("p (h e) -> p h e", e=129)
        for c in range(16):
            tsl = slice(c * 128, (c + 1) * 128)
            pq = bank()
            for h in range(4):
                for k in range(8):
                    mm(pq[:, h * 128:(h + 1) * 128], WS[0][:, k, h * 128:(h + 1) * 128], XNT[:, k, tsl], k == 0, k == 7)
            cp(QT, pq[:, :].rearrange("p (h d) -> p h d", d=128), eng="act")
            pkk = bank()
            for h in range(4):
                for k in range(8):
                    mm(pkk[:, h * 128:(h + 1) * 128], WS[3][:, k, h * 128:(h + 1) * 128], XNT[:, k, tsl], k == 0, k == 7)
            cp(KT, pkk[:, :].rearrange("p (h d) -> p h d", d=128), eng="act")
            pkt = bank()
            for k in range(8):
                mm(pkt[:, :], XNT[:, k, tsl], WS[3][:, k, :], k == 0, k == 7)
            stt(WK, pkt[:, :].rearrange("p (h d) -> p h d", d=128), KSC,
                v3(WKB)[:, c, :].unsqueeze(2).to_broadcast([128, 4, 128]), ALU.mult, ALU.mult)
            pv_ = bank()
            for k in range(8):
                mm(pv_[:, :], XNT[:, k, tsl], WS[4][:, k, :], k == 0, k == 7)
            cp(VX[:, :, 0:128], pv_[:, :].rearrange("p (h e) -> p h e", e=128), eng="act")
            po_ = bank()
            for k in range(8):
                mm(po_[:, :], XNT[:, k, tsl], WS[1][:, k, :], k == 0, k == 7)
            act(OG, po_[:, :], AF.Sigmoid)
            tt(OG, OG, GHEAD, ALU.mult)
            tt(RC.rearrange("p (h t) -> p h t", t=128), NEGMR.unsqueeze(1).to_broadcast([64, 4, 128]),
               IDF[0:64, 4 * c:4 * c + 4].unsqueeze(2).to_broadcast([64, 4, 128]), ALU.mult)
            pe_ = bank()
            mm(pe_[:, :], IDB, MNEG, True, False)
            mm(pe_[:, :], ONES[0:64, :], RC, False, True)
            ps_ = bank()
            for h in range(4):
                mm(ps_[:, h * 128:(h + 1) * 128], KT[:, h, :], QT[:, h, :])
            for h in range(4):
                act(DD[:, h, :], pe_[:, h * 128:(h + 1) * 128], AF.Exp, bias=AC[:, c * 4 + h:c * 4 + h + 1])
            stt(PP, ps_[:, :].rearrange("p (h d) -> p h d", d=128), KSC, DD, ALU.mult, ALU.mult)
            pO = [bank(), bank()]
            pQ = [bank(), bank()]
            for h in range(4):
                mm(pO[h // 2][:, (h % 2) * 129:(h % 2) * 129 + 129], PP[:, h, :], VX[:, h, 0:129])
            for h in range(4):
                mm(pQ[h // 2][:, (h % 2) * 129:(h % 2) * 129 + 129], QT[:, h, :], CBF[:, h, 0:129])
            for h in range(4):
                act(QCS[:, h * 129:(h + 1) * 129], pQ[h // 2][:, (h % 2) * 129:(h % 2) * 129 + 129], AF.Identity,
                    scale=INTER[:, c * 4 + h:c * 4 + h + 1])
            for hh in range(2):
                tt(ND[:, hh * 258:(hh + 1) * 258], pO[hh][:, 0:258], QCS[:, hh * 258:(hh + 1) * 258], ALU.add)
            ts(NDEN, ND3[:, :, 128], -1.0, None, ALU.mult)
            tt(DEN, ND3[:, :, 128], NDEN, ALU.max)
            tt(DEN, DEN, v3(FL)[:, c, :], ALU.max)
            recip(RDEN, DEN)
            HV = ND3[:, :, 0:128]
            tt(HV, HV, RDEN.unsqueeze(2).to_broadcast([128, 4, 128]), ALU.mult)
            for h in range(4):
                stt(JNK, ND3[:, h, 0:128], 1.0, ND3[:, h, 0:128], ALU.mult, ALU.mult, accum=SSH[:, h:h + 1])
            ts(RSH, SSH, 1.0 / 128, EPS, ALU.mult, ALU.add)
            act(RSH, RSH, AF.Sqrt)
            recip(RSH, RSH)
            tt(HV, HV, RSH.unsqueeze(2).to_broadcast([128, 4, 128]), ALU.mult)
            tt(YB.rearrange("p (h d) -> p h d", d=128), HV, OG.rearrange("p (h d) -> p h d", d=128), ALU.mult)
            pT = bank()
            pTb = pT[:, :].bitcast(BF16)
            for h in range(4):
                tr(pTb[:, h * 128:(h + 1) * 128], YB[:, h * 128:(h + 1) * 128], IDB)
            cp(YBT, pTb[:, 0:512].rearrange("p (h d) -> p h d", d=128), eng="act")
            for half in range(2):
                po = bank()
                for h in range(4):
                    mm(po[:, :], YBT[:, h, :], WOUT[:, 4 + h, half * 512:(half + 1) * 512], h == 0, h == 3)
                resid_add(c, half, po[:, :])
            pKV = [bank(), bank()]
            for h in range(4):
                mm(pKV[h // 2][:, (h % 2) * 129:(h % 2) * 129 + 129], WK[:, h, :], VX[:, h, 0:129])
            for h in range(4):
                stt(CST_[:, h, :], CST_[:, h, :], DEC[:, c * 4 + h:c * 4 + h + 1],
                    pKV[h // 2][:, (h % 2) * 129:(h % 2) * 129 + 129], ALU.mult, ALU.add)
            cp(CBF[:, :, 0:129], CST_, eng="act")
    K.even_mixer = even_mixer


    for kind, l in stages:
        if kind == "even":
            norm_to_xnt(gmix_d[l], 0)
            even_mixer(l, l // 2)
        elif kind == "odd":
            norm_to_xnt(gmix_d[l], 0)
            odd_mixer(l // 2)
        elif kind == "ffn":
            norm_to_xnt(gffn_d[l], 1)
            ffn(l)
    yout = y_d.rearrange("(t p) d -> p t d", p=128)
    if final_norm:
        g_b = GB[0]
        dma("sp", "gb0", [(g_b, gfin_d)])
        for t in range(NT):
            act(XS[t % 2], X[:, t, :], AF.Square, accum=SSQ[:, t:t + 1])
        ts(RSTD, SSQ, 1.0 / D, EPS, ALU.mult, ALU.add)
        act(RSTD, RSTD, AF.Sqrt)
        recip(RSTD, RSTD)
        for t in range(NT):
            stt(X[:, t, :], X[:, t, :], RSTD[:, t:t + 1], g_b, ALU.mult, ALU.mult)
    for q4 in range(4):
        dma("sp", "yout", [(yout[:, q4 * 4:(q4 + 1) * 4, :], X[:, q4 * 4:(q4 + 1) * 4, :])])
    n_out = P.dma_sems["yout"]
    block = es.enter_context(nc.Block())

    def fin(h):
        h.wait_ge(sems_dma["yout"], n_out)
    P.emit(block, sems_eng, sems_dma, extra={"sp": fin})
    es.close()
    return nc


def _consts():
    s = np.arange(128)[:, None]
    t = np.arange(128)[None, :]
    ident = np.eye(128, dtype=np.float32)
    mst = (s <= t).astype(np.float32)
    ones = np.ones((128, 128), np.float32)
    c_f32 = np.concatenate([ident, mst, ones], axis=1)
    mneg = np.where(s <= t, 0.0, -30000.0).astype(np.float32)
    c_bf = np.concatenate([ident, np.tile(mneg, (1, 4))], axis=1)
    return np.ascontiguousarray(c_f32), np.ascontiguousarray(c_bf)


def make_in_maps(inp):
    f = lambda a: np.ascontiguousarray(np.asarray(a, dtype=np.float32))
    rep = lambda a: np.ascontiguousarray(np.broadcast_to(np.asarray(a, np.float32)[..., None, :], a.shape[:-1] + (128, a.shape[-1])))
    c_f32, c_bf = _consts()
    wc = np.asarray(inp["even_w_conv"], np.float32)
    wconvT = np.ascontiguousarray(wc.reshape(2, 3, 4, 128).transpose(0, 3, 2, 1).reshape(2, 128, 12))
    bs = np.asarray(inp["odd_b_s"], np.float32).reshape(2, 1024)
    shared = {
        "c_f32": c_f32, "c_bf": c_bf,
        "gmix_b": rep(inp["norm_mix"]), "gffn_b": rep(inp["norm_ffn"]), "gfin_b": rep(inp["norm_final"]),
        "bgate_b": rep(inp["even_b_gate"]), "wconvT": wconvT, "ghead_b": rep(inp["even_g_head"]),
        "gv_b": rep(inp["odd_g_v"]), "bs_b": rep(bs),
        "even_w_in": f(inp["even_w_in"]), "even_w_out": f(inp["even_w_out"]),
        "odd_w_in": f(inp["odd_w_in"]), "odd_w_s": f(inp["odd_w_s"]), "odd_w_out": f(inp["odd_w_out"]),
        "ffn_w_in": f(inp["ffn_w_in"]), "ffn_w_out": f(inp["ffn_w_out"]),
    }
    x = np.asarray(inp["x"], np.float32)
    maps = []
    for c in range(8):
        b, sg = c // 4, c % 4
        fl = np.zeros((128, 12), np.float32)
        for j in range(4):
            inc = 1.0 if j < sg else 0.0
            fl[:, j] = inc
            fl[:, 4 + j] = (inc - 1.0) * 1e30
            fl[:, 8 + j] = 1.0 if j == sg - 1 else 0.0
        m = dict(shared)
        m["x"] = np.ascontiguousarray(x[b, sg * TOK:(sg + 1) * TOK])
        m["flags"] = fl
        maps.append(m)
    return maps


def run_stages(inp, stages, final_norm):
    nc = build_program(stages, final_norm)
    maps = make_in_maps(inp)
    res = run_bass_kernel_spmd(nc, maps, core_ids=list(range(8)))
    out = np.empty((2, 8192, 1024), np.float32)
    for c in range(8):
        out[c // 4, (c % 4) * TOK:(c % 4 + 1) * TOK] = res.results[c]["y"]
    return out


def kernel(**inputs):
    stages = []
    for l in range(4):
        stages.append(("even" if l % 2 == 0 else "odd", l))
        stages.append(("ffn", l))
    return run_stages(inputs, stages, True)
```

```python
from contextlib import ExitStack
from concourse.bass_utils import run_bass_kernel_spmd
import numpy as np
import concourse.bass as bass
import concourse.mybir as mybir

F32 = mybir.dt.float32
BF16 = mybir.dt.bfloat16
ALU = mybir.AluOpType
AF = mybir.ActivationFunctionType
AX = mybir.AxisListType


import os
STRICT = bool(os.environ.get('BASS_STRICT'))


class _Op:
    __slots__ = ("eng", "idx", "fn", "waits", "signal", "dma_sem", "dma_cnt", "sigval", "isdma")

    def __init__(self, eng, idx, fn):
        self.eng = eng
        self.idx = idx
        self.fn = fn
        self.waits = []
        self.signal = False
        self.dma_sem = None
        self.dma_cnt = 0
        self.sigval = 0
        self.isdma = False


def _ap_range(ap):
    sp = str(ap.space)
    name = ap.tensor.name
    if "DRAM" in sp.upper() or sp.upper() not in ("SB", "PSUM"):
        return ("D:" + name, 0, 1)
    esz = mybir.dt.size(ap.dtype)
    pat = ap.ap
    pstride = pat[0][0]
    off = ap.offset % pstride if pstride > 0 else ap.offset
    ext = 0
    for st, cnt in pat[1:]:
        ext += abs(st) * (cnt - 1)
    lo = off * esz
    hi = (off + ext + 1) * esz
    if sp.upper() == "PSUM":
        lo, hi = (lo // 2048) * 2048, ((hi + 2047) // 2048) * 2048
    return (sp + ":" + name, lo, hi)


class Prog:
    ENGS = ("pe", "act", "dve", "pool", "sp")

    def __init__(self, nc):
        self.nc = nc
        self.ops = {e: [] for e in self.ENGS}
        self.segs = {}
        self.seen = {e: {f: -1 for f in self.ENGS} for e in self.ENGS}
        self.seen_sem = {e: {} for e in self.ENGS}
        self.dma_sems = {}
        self.extra_sems = []

    def _touch(self, key, lo, hi, op, is_write, deps):
        segs = self.segs.setdefault(key, [])
        new = []
        for s in segs:
            if s[1] <= lo or s[0] >= hi:
                new.append(s)
                continue
            cuts = [s[0]] + [c for c in (lo, hi) if s[0] < c < s[1]] + [s[1]]
            for a, b in zip(cuts[:-1], cuts[1:]):
                new.append([a, b, s[2], dict(s[3])])
        new.sort(key=lambda s: s[0])
        out = []
        cur = lo
        for s in new:
            if s[1] <= lo or s[0] >= hi:
                out.append(s)
                continue
            if s[0] > cur:
                out.append([cur, s[0], None, {}])
            out.append(s)
            cur = s[1]
        if cur < hi:
            out.append([cur, hi, None, {}])
        out.sort(key=lambda s: s[0])
        for s in out:
            if s[1] <= lo or s[0] >= hi:
                continue
            if s[2] is not None:
                deps.append((s[2], "raw" if not is_write else "waw"))
            if is_write:
                for r in s[3].values():
                    deps.append((r, "war"))
                s[2] = op
                s[3] = {}
            else:
                s[3][op.eng if op.dma_sem is None and not op.isdma else ("dma", id(op))] = op
        merged = []
        for s in out:
            if merged and merged[-1][1] == s[0] and merged[-1][2] is s[2] and merged[-1][3] == s[3]:
                merged[-1][1] = s[1]
            else:
                merged.append(s)
        self.segs[key] = merged

    def add(self, eng, fn, reads=(), writes=(), dma_slot=None, n_dma=1):
        op = _Op(eng, len(self.ops[eng]), fn)
        op.isdma = dma_slot is not None
        deps = []
        for ap in reads:
            k, lo, hi = ap if isinstance(ap, tuple) else _ap_range(ap)
            self._touch(k, lo, hi, op, False, deps)
        for ap in writes:
            k, lo, hi = ap if isinstance(ap, tuple) else _ap_range(ap)
            self._touch(k, lo, hi, op, True, deps)
        if dma_slot is not None:
            self.dma_sems[dma_slot] = self.dma_sems.get(dma_slot, 0) + 16 * n_dma
            op.dma_sem = dma_slot
            op.dma_cnt = self.dma_sems[dma_slot]
        best = {}
        for dop, kind in deps:
            if dop is op:
                continue
            if dop.dma_sem is not None:
                cur = self.seen_sem[eng].get(dop.dma_sem, 0)
                if dop.dma_cnt > cur:
                    self.seen_sem[eng][dop.dma_sem] = dop.dma_cnt
                    op.waits.append(("sem", dop.dma_sem, dop.dma_cnt))
                continue
            if dop.eng == eng and not op.isdma:
                if eng == "pe" or (kind != "raw" and not STRICT):
                    continue
            if dop.idx > best.get(dop.eng, -1):
                best[dop.eng] = dop.idx
        for f, idx in best.items():
            if idx > self.seen[eng][f]:
                self.seen[eng][f] = idx
                self.ops[f][idx].signal = True
                op.waits.append(("eng", f, idx))
        self.ops[eng].append(op)
        return op

    def emit(self, block, sems_eng, sems_dma, extra=None):
        nc = self.nc
        for e in self.ENGS:
            c = 0
            for op in self.ops[e]:
                if op.signal:
                    c += 1
                    op.sigval = c
        handles = {"pe": nc.tensor, "act": nc.scalar, "dve": nc.vector, "pool": nc.gpsimd, "sp": nc.sync}

        def body(e):
            def f(h):
                for op in self.ops[e]:
                    for w in op.waits:
                        if w[0] == "sem":
                            h.wait_ge(sems_dma[w[1]], w[2])
                        else:
                            h.wait_ge(sems_eng[w[1]], self.ops[w[1]][w[2]].sigval)
                    ins = op.fn(h)
                    if op.dma_sem is not None:
                        pass
                    elif op.signal:
                        ins.then_inc(sems_eng[e], 1)
                if extra and e in extra:
                    extra[e](h)
            return f
        block.tensor(body("pe"))
        block.scalar(body("act"))
        block.vector(body("dve"))
        block.gpsimd(body("pool"))
        block.sync(body("sp"))


NT = 16
TOK = 2048
D = 1024
EPS = 1e-6
KSC = 128 ** -0.5
SUMW = 532

O_X = 0
O_XNT = 65536
O_CST = 98304
O_GB = O_CST + 8192
O_XS = O_GB + 8192
O_PH = O_XS + 4096
PH_SIZE = 90112
ARENA = O_PH + PH_SIZE


class K:
    pass


def build_program(stages, final_norm=True):
    nc = bass.Bass("TRN2", target_bir_lowering=False)
    P = Prog(nc)
    dt_in = {}

    def din(name, shape):
        t = nc.dram_tensor(name, list(shape), F32, kind="ExternalInput")
        dt_in[name] = t
        return t.ap()

    x_d = din("x", [TOK, D])
    cf_d = din("c_f32", [128, 384])
    cb_d = din("c_bf", [128, 640])
    fl_d = din("flags", [128, 12])
    gmix_d = din("gmix_b", [4, 128, D])
    gffn_d = din("gffn_b", [4, 128, D])
    gfin_d = din("gfin_b", [128, D])
    bgate_d = din("bgate_b", [2, 128, 8])
    wconv_d = din("wconvT", [2, 128, 12])
    ghead_d = din("ghead_b", [2, 128, 512])
    gv_d = din("gv_b", [2, 128, D])
    bs_d = din("bs_b", [2, 128, D])
    ewin_d = din("even_w_in", [2, 1024, 3592])
    ewout_d = din("even_w_out", [2, 1024, 1024])
    owin_d = din("odd_w_in", [2, 1024, 2048])
    ows_d = din("odd_w_s", [2, 8, 128, 128])
    owout_d = din("odd_w_out", [2, 1024, 1024])
    fwin_d = din("ffn_w_in", [4, 1024, 5632])
    fwout_d = din("ffn_w_out", [4, 2816, 1024])
    y_d = nc.dram_tensor("y", [TOK, D], F32, kind="ExternalOutput").ap()
    ccin = [nc.dram_tensor(f"ccin{i}", [128, SUMW], F32) for i in range(2)]
    ccout = [nc.dram_tensor(f"ccout{i}", [512, SUMW], F32) for i in range(2)]

    es = ExitStack()
    A = es.enter_context(nc.sbuf_tensor("arena", [128, ARENA // 4], F32))
    PS = [es.enter_context(nc.psum_tensor(f"ps{i}", [128, 512], F32)) for i in range(8)]
    sems_eng = {e: es.enter_context(nc.semaphore("s_" + e)) for e in Prog.ENGS}
    sems_dma = {}

    def dsem(name):
        if name not in sems_dma:
            sems_dma[name] = es.enter_context(nc.semaphore("d_" + name))
        return sems_dma[name]

    def f32v(off, n, parts=128):
        return A[0:parts, off // 4: off // 4 + n]

    def bfv(off, n, parts=128):
        return A[0:parts, off // 4: off // 4 + (n + 1) // 2].bitcast(BF16)[:, 0:n]

    X = f32v(O_X, NT * D).rearrange("p (t d) -> p t d", d=D)
    XNT = bfv(O_XNT, 8 * TOK).rearrange("p (k t) -> p k t", t=TOK)
    c = O_CST
    IDF = f32v(c, 128); c += 512
    MST = f32v(c, 128); c += 512
    ONES = f32v(c, 128); c += 512
    IDB = bfv(c, 128); c += 256
    MNEG = bfv(c, 512); c += 1024
    FLG = f32v(c, 12); c += 48
    SSQ = f32v(c, 16); c += 64
    RSTD = f32v(c, 16); c += 64
    BGATE = f32v(c, 8); c += 32
    WCONV = f32v(c, 12); c += 48
    SM = f32v(c, 64); c += 256
    ZER = f32v(c, 128); c += 512
    GHEAD = f32v(c, 512); c += 2048
    NEGH = f32v(c, 16); c += 64
    assert c <= O_CST + 8192, c
    GB = [f32v(O_GB + i * 4096, D) for i in range(2)]
    XS = [bfv(O_XS + i * 2048, D) for i in range(2)]

    def mm(out, lhsT, rhs, start=True, stop=True):
        P.add("pe", lambda h, o=out, l=lhsT, r=rhs, s=start, e=stop: h.matmul(o, l, r, start=s, stop=e),
              reads=[lhsT, rhs], writes=[out])

    def tr(out, in_, ident):
        P.add("pe", lambda h, o=out, i=in_, d=ident: h.transpose(o, i, d), reads=[in_, ident], writes=[out])

    def act(out, in_, func, bias=None, scale=None, accum=None):
        rd = [in_] + [v for v in (bias, scale) if v is not None and not isinstance(v, (int, float))]
        wr = [out] + ([accum] if accum is not None else [])
        kw = {}
        if bias is not None:
            kw["bias"] = bias
        if scale is not None:
            kw["scale"] = scale
        if accum is not None:
            kw["accum_out"] = accum
        P.add("act", lambda h, o=out, i=in_, f=func, k=kw: h.activation(out=o, in_=i, func=f, **k), reads=rd, writes=wr)

    def tt(out, in0, in1, op, eng="dve"):
        P.add(eng, lambda h, o=out, a=in0, b=in1, p=op: h.tensor_tensor(out=o, in0=a, in1=b, op=p),
              reads=[in0, in1], writes=[out])

    def ts(out, in0, s1, s2, op0, op1=None, eng="dve"):
        rd = [in0] + [v for v in (s1, s2) if v is not None and not isinstance(v, (int, float))]
        if op1 is None:
            P.add(eng, lambda h, o=out, a=in0, x=s1, p=op0: h.tensor_scalar(out=o, in0=a, scalar1=x, scalar2=None, op0=p),
                  reads=rd, writes=[out])
        else:
            P.add(eng, lambda h, o=out, a=in0, x=s1, y=s2, p=op0, q=op1: h.tensor_scalar(out=o, in0=a, scalar1=x, scalar2=y, op0=p, op1=q),
                  reads=rd, writes=[out])

    def stt(out, in0, scalar, in1, op0, op1, accum=None):
        rd = [in0, in1] + ([scalar] if not isinstance(scalar, (int, float)) else [])
        wr = [out] + ([accum] if accum is not None else [])
        if accum is None:
            P.add("dve", lambda h, o=out, a=in0, s=scalar, b=in1, p=op0, q=op1: h.scalar_tensor_tensor(out=o, in0=a, scalar=s, in1=b, op0=p, op1=q),
                  reads=rd, writes=wr)
        else:
            P.add("dve", lambda h, o=out, a=in0, s=scalar, b=in1, p=op0, q=op1, ac=accum: h.scalar_tensor_tensor(out=o, in0=a, scalar=s, in1=b, op0=p, op1=q, accum_out=ac),
                  reads=rd, writes=wr)

    def cp(out, in_, eng="dve"):
        if eng == "act":
            act(out, in_, AF.Copy)
        else:
            P.add(eng, lambda h, o=out, i=in_: h.tensor_copy(out=o, in_=i), reads=[in_], writes=[out])

    def scan(out, d0, d1, init, op0, op1):
        rd = [d0, d1] + ([init] if not isinstance(init, (int, float)) else [])
        P.add("dve", lambda h, o=out, a=d0, b=d1, i=init, p=op0, q=op1: h.tensor_tensor_scan(out=o, data0=a, data1=b, initial=i, op0=p, op1=q),
              reads=rd, writes=[out])

    def red(out, in_, op):
        P.add("dve", lambda h, o=out, i=in_, p=op: h.tensor_reduce(out=o, in_=i, axis=AX.X, op=p), reads=[in_], writes=[out])

    def recip(out, in_):
        P.add("dve", lambda h, o=out, i=in_: h.reciprocal(out=o, in_=i), reads=[in_], writes=[out])

    def memset(out, val, eng="pool"):
        P.add(eng, lambda h, o=out, v=val: h.memset(o, v), writes=[out])

    def dma(queue, slot, pairs, slot_ap=None, extra_reads=()):
        sem = dsem(slot)
        def fn(h, pairs=pairs, sem=sem):
            r = None
            for o, i in pairs:
                r = h.dma_start(out=o, in_=i).then_inc(sem, 16)
            return r
        wr = [slot_ap] if slot_ap is not None else [o for o, _ in pairs]
        P.add(queue, fn, reads=[i for _, i in pairs] + list(extra_reads), writes=wr, dma_slot=slot, n_dma=len(pairs))

    def rstd_op(out, ss, scale, n):
        ts(out, ss, scale, EPS, ALU.mult, ALU.add)
        P.add("pool", lambda h, o=out, nh=NEGH[:, 0:n]: h.tensor_tensor(out=o, in0=o, in1=nh, op=ALU.pow),
              reads=[out, NEGH[:, 0:n]], writes=[out])

    _bank = [0]

    def bank():
        b = PS[_bank[0] % 8]
        _bank[0] += 1
        return b

    dma("sp", "cst", [(A[:, O_CST // 4: O_CST // 4 + 384], cf_d), (FLG, fl_d)], slot_ap=A[:, O_CST // 4: (O_CST + 8192) // 4])
    dma("pool", "cstb", [(bfv(O_CST + 1536, 640), cb_d)], slot_ap=bfv(O_CST + 1536, 640))
    memset(ZER, 0.0)
    memset(NEGH, -0.5)
    xin = x_d.rearrange("(t p) d -> p t d", p=128)
    for q4 in range(4):
        dma("sp", f"xin{q4}", [(X[:, q4 * 4:(q4 + 1) * 4, :], xin[:, q4 * 4:(q4 + 1) * 4, :])])

    def norm_to_xnt(g_src, gslot):
        g_b = GB[gslot]
        dma("sp", f"gb{gslot}", [(g_b, g_src)])
        for t in range(NT):
            act(XS[t % 2], X[:, t, :], AF.Square, accum=SSQ[:, t:t + 1])
        rstd_op(RSTD, SSQ, 1.0 / D, 16)
        for t in range(NT):
            xs = XS[t % 2]
            stt(xs, X[:, t, :], RSTD[:, t:t + 1], g_b, ALU.mult, ALU.mult)
            pb = bank()
            pbb = pb[:, :].bitcast(BF16)
            for k in range(8):
                tr(pbb[:, k * 128:(k + 1) * 128], xs[:, k * 128:(k + 1) * 128], IDB)
            cp(XNT[:, :, t * 128:(t + 1) * 128], pbb.rearrange("p (k t) -> p k t", t=128), eng="act")

    def resid_add(t, half, ps):
        xv = X[:, t, half * 512:(half + 1) * 512]
        tt(xv, ps, xv, ALU.add)

    def ffn(l):
        groups = [(0, 6), (6, 6), (12, 5), (17, 5)]
        WI = [bfv(O_PH + s * 36864, 8 * 2 * 768).rearrange("p (k u n) -> p k u n", u=2, n=768) for s in range(2)]
        WO = [bfv(O_PH + s * 36864 + 24576, 6 * 1024).rearrange("p (j d) -> p j d", d=1024) for s in range(2)]
        ACTB = [bfv(O_PH + 73728 + s * 6144, 6 * 512).rearrange("p (j t) -> p j t", t=512) for s in range(2)]
        SG = [f32v(O_PH + 86016 + s * 2048, 512) for s in range(2)]
        it = 0
        for gi, (c0, n) in enumerate(groups):
            s = gi % 2
            slot_ap = bfv(O_PH + s * 36864, 18432)
            win = fwin_d[l].rearrange("(k p) n -> p k n", p=128)
            dma("pool", f"ffw{s}", [
                (WI[s][:, :, 0, 0:n * 128], win[:, :, c0 * 128:(c0 + n) * 128]),
                (WI[s][:, :, 1, 0:n * 128], win[:, :, 2816 + c0 * 128:2816 + (c0 + n) * 128]),
                (WO[s][:, 0:n, :], fwout_d[l][c0 * 128:(c0 + n) * 128, :].rearrange("(j p) d -> p j d", p=128)),
            ], slot_ap=slot_ap)
            for b in range(4):
                ab = ACTB[it % 2]
                it += 1
                for j in range(n):
                    pg = bank()
                    pu = bank()
                    for k in range(8):
                        mm(pg[:, :], WI[s][:, k, 0, j * 128:(j + 1) * 128], XNT[:, k, b * 512:(b + 1) * 512], k == 0, k == 7)
                    for k in range(8):
                        mm(pu[:, :], WI[s][:, k, 1, j * 128:(j + 1) * 128], XNT[:, k, b * 512:(b + 1) * 512], k == 0, k == 7)
                    sg = SG[j % 2]
                    act(sg, pg[:, :], AF.Silu)
                    tt(ab[:, j, :], pu[:, :], sg, ALU.mult)
                for i in range(4):
                    t = b * 4 + i
                    for half in range(2):
                        po = bank()
                        for j in range(n):
                            mm(po[:, :], ab[:, j, i * 128:(i + 1) * 128], WO[s][:, j, half * 512:(half + 1) * 512], j == 0, j == n - 1)
                        resid_add(t, half, po[:, :])

    def odd_mixer(j):
        o = O_PH
        WU = bfv(o, 8 * 1024).rearrange("p (k n) -> p k n", n=1024); o += 16384
        WV = bfv(o, 8 * 1024).rearrange("p (k n) -> p k n", n=1024); o += 16384
        WOUT = bfv(o, 8 * 1024).rearrange("p (k n) -> p k n", n=1024); o += 16384
        WST = bfv(o, 8 * 128).rearrange("p (g t) -> p g t", t=128); o += 2048
        BSB = f32v(o, 1024); o += 4096
        GVB = f32v(o, 1024); o += 4096
        UT = bfv(o, 8 * 512).rearrange("p (g t) -> p g t", t=512); o += 8192
        VG = f32v(o, 1024); o += 4096
        VN = bfv(o, 1024); o += 2048
        T1 = f32v(o, 1024); o += 4096
        GT = bfv(o, 1024).rearrange("p (g t) -> p g t", t=128); o += 2048
        WSS = f32v(o, 1024).rearrange("p (g s) -> p g s", s=128); o += 4096
        JNK = bfv(o, 1024); o += 2048
        VNB = bfv(o, 1024); o += 2048
        assert o <= O_PH + PH_SIZE
        UT2 = bfv(O_GB, 8 * 512).rearrange("p (g t) -> p g t", t=512)
        win = owin_d[j].rearrange("(k p) n -> p k n", p=128)
        dma("pool", "owu", [(WU, win[:, :, 0:1024])])
        dma("pool", "owv", [(WV, win[:, :, 1024:2048])])
        dma("pool", "owo", [(WOUT, owout_d[j].rearrange("(k p) n -> p k n", p=128))])
        dma("sp", "ows", [(WSS, ows_d[j].rearrange("g t s -> t g s"))])
        dma("sp", "obs", [(BSB, bs_d[j])])
        dma("sp", "ogv", [(GVB, gv_d[j])])
        for g in range(8):
            pb = bank()
            tr(pb[:, 0:128], WSS[:, g, :], IDF)
            tt(WST[:, g, :], pb[:, 0:128], MST, ALU.mult)
        VN2 = [VN, VNB]
        UTB = [UT, UT2]

        def stage_a(t):
            vn = VN2[t % 2]
            pv = [bank(), bank()]
            for half in range(2):
                for k in range(8):
                    mm(pv[half][:, :], XNT[:, k, t * 128:(t + 1) * 128], WV[:, k, half * 512:(half + 1) * 512], k == 0, k == 7)
            for half in range(2):
                act(VG[:, half * 512:(half + 1) * 512], pv[half][:, :], AF.Gelu_apprx_tanh)
            ss = SM[:, 0:1]
            stt(JNK, VG, 1.0, VG, ALU.mult, ALU.mult, accum=ss)
            rs = SM[:, 1:2]
            rstd_op(rs, ss, 1.0 / 1024, 1)
            stt(vn, VG, rs, GVB, ALU.mult, ALU.mult)

        def stage_b1(t):
            vn = VN2[t % 2]
            b, i = t // 4, t % 4
            ut = UTB[b % 2]
            pg = [bank(), bank()]
            for g in range(8):
                mm(pg[g // 4][:, (g % 4) * 128:(g % 4 + 1) * 128], vn[:, g * 128:(g + 1) * 128], WST[:, g, :])
            for hf in range(2):
                tt(T1[:, hf * 512:(hf + 1) * 512], pg[hf][:, :], BSB[:, hf * 512:(hf + 1) * 512], ALU.add)
            tt(GT, T1.rearrange("p (g t) -> p g t", t=128), ut[:, :, i * 128:(i + 1) * 128], ALU.mult)

        def stage_b2(t):
            for half in range(2):
                po = bank()
                for g in range(8):
                    mm(po[:, :], GT[:, g, :], WOUT[:, g, half * 512:(half + 1) * 512], g == 0, g == 7)
                resid_add(t, half, po[:, :])

        def ublock(b):
            for fc in range(8):
                pb = bank()
                for k in range(8):
                    mm(pb[:, :], WU[:, k, fc * 128:(fc + 1) * 128], XNT[:, k, b * 512:(b + 1) * 512], k == 0, k == 7)
                act(UTB[b % 2][:, fc, :], pb[:, :], AF.Gelu_apprx_tanh)

        ublock(0)
        stage_a(0)
        for t in range(16):
            if t + 1 < 16:
                if (t + 1) % 4 == 0:
                    ublock((t + 1) // 4)
                stage_a(t + 1)
            stage_b1(t)
            stage_b2(t)

    K.norm_to_xnt = norm_to_xnt
    K.ffn = ffn
    K.odd_mixer = odd_mixer
    class Bump:
        def __init__(self, base, limit):
            self.base, self.limit, self.cur = base, limit, base

        def reset(self):
            self.cur = self.base

        def _take(self, nbytes):
            o = self.cur
            self.cur += (nbytes + 31) // 32 * 32
            assert self.cur <= self.limit, (self.cur, self.limit)
            return o

        def f32(self, n, parts=128):
            return f32v(self._take(n * 4), n, parts)

        def bf(self, n, parts=128):
            return bfv(self._take(n * 2), n, parts)

    K.reserved = set()
    _obank = bank

    def bank():
        while True:
            i = _bank[0] % 8
            _bank[0] += 1
            if i not in K.reserved:
                return PS[i]

    def even_mixer(l, j):
        cci = j
        ws_off = [O_PH + i * 8192 for i in range(5)]
        WS = [bfv(ws_off[i], 8 * 512).rearrange("p (k n) -> p k n", n=512) for i in range(5)]
        WOUT = bfv(O_PH + 40960, 8 * 1024).rearrange("p (k n) -> p k n", n=1024)
        sm = Bump(O_PH + 57344, O_PH + 71680)
        tp = Bump(O_PH + 71680, O_PH + PH_SIZE)
        s2 = Bump(ws_off[2], ws_off[2] + 8192)
        win = ewin_d[j].rearrange("(k p) n -> p k n", p=128)

        def loadw(slot, col0):
            dma("pool", f"ew{slot}", [(WS[slot], win[:, :, col0:col0 + 512])])
        loadw(0, 0); loadw(1, 512); loadw(2, 1024)
        dma("pool", "ewo", [(WOUT, ewout_d[j].rearrange("(k p) n -> p k n", p=128))])
        loadw(3, 2048); loadw(4, 2560)
        WG = sm.bf(64).rearrange("p (k n) -> p k n", n=8)
        dma("pool", "ewg", [(WG, win[:, :, 3584:3592])])
        dma("sp", "ebg", [(BGATE, bgate_d[j])])
        dma("sp", "ewc", [(WCONV, wconv_d[j])])
        dma("sp", "egh", [(GHEAD, ghead_d[j])])
        v3 = lambda ap: ap.rearrange("p (c h) -> p c h", h=4)
        ZH = sm.f32(8).rearrange("p (c t) -> p c t", t=2)
        Z01 = sm.f32(8).rearrange("p (c t) -> p c t", t=2)
        BG01 = sm.f32(8).rearrange("p (c t) -> p c t", t=2)
        SUMM = sm.f32(SUMW)
        WC = WCONV.rearrange("p (c j) -> p c j", j=3)
        tp.reset()
        YAB = tp.bf(4 * 512).rearrange("p (c t) -> p c t", t=512)
        XA = tp.f32(512)
        Z = [tp.f32(514) for _ in range(2)]
        ACC = tp.f32(512)
        memset(ZH, 0.0)
        it = 0
        for b in range(4):
            for cc in range(4):
                pbg, pcg, pxa = bank(), bank(), bank()
                for pp, sl in ((pbg, 0), (pcg, 1), (pxa, 2)):
                    for k in range(8):
                        mm(pp[:, :], WS[sl][:, k, cc * 128:(cc + 1) * 128], XNT[:, k, b * 512:(b + 1) * 512], k == 0, k == 7)
                z = Z[it % 2]
                it += 1
                cp(XA, pxa[:, :], eng="act")
                cp(z[:, 0:2], ZH[:, cc, :], eng="pool")
                tt(z[:, 2:514], pcg[:, :], XA, ALU.mult)
                ts(ACC, z[:, 2:514], WC[:, cc, 2:3], None, ALU.mult)
                stt(ACC, z[:, 1:513], WC[:, cc, 1:2], ACC, ALU.mult, ALU.add)
                stt(ACC, z[:, 0:512], WC[:, cc, 0:1], ACC, ALU.mult, ALU.add)
                tt(YAB[:, cc, :], pbg[:, :], ACC, ALU.mult)
                cp(ZH[:, cc, :], z[:, 512:514], eng="pool")
                if b == 0:
                    cp(Z01[:, cc, :], z[:, 2:4], eng="pool")
                    cp(BG01[:, cc, :], pbg[:, 0:2], eng="act")
                if b == 3:
                    cp(SUMM[:, 524 + cc * 2:526 + cc * 2], z[:, 512:514], eng="pool")
            for i in range(4):
                t = b * 4 + i
                for half in range(2):
                    po = bank()
                    for cc in range(4):
                        mm(po[:, :], YAB[:, cc, i * 128:(i + 1) * 128], WOUT[:, cc, half * 512:(half + 1) * 512], cc == 0, cc == 3)
                    resid_add(t, half, po[:, :])
        loadw(0, 1536)
        loadw(1, 3072)
        GC = sm.f32(128).rearrange("p (t n) -> p t n", n=8)
        pgt = bank()
        for t in range(16):
            for k in range(8):
                mm(pgt[:, t * 8:(t + 1) * 8], XNT[:, k, t * 128:(t + 1) * 128], WG[:, k, :], k == 0, k == 7)
        tt(GC, pgt[:, 0:128].rearrange("p (t n) -> p t n", n=8), BGATE.unsqueeze(1).to_broadcast([128, 16, 8]), ALU.add)
        IC = sm.f32(64); LFC = sm.f32(64); EX = sm.f32(64)
        cp(v3(IC), GC[:, :, 0:4])
        act(v3(EX), GC[:, :, 4:8], AF.Exp, scale=-1.0)
        act(EX, EX, AF.Ln, bias=1.0)
        ts(LFC, EX, -1.0, None, ALU.mult)
        pr = bank()
        tr(pr[0:64, 0:128], IC, IDF)
        tr(pr[0:64, 128:256], LFC, IDF)
        IR = sm.f32(128, 64); LFR = sm.f32(128, 64); BR = sm.f32(128, 64); AR = sm.f32(128, 64)
        CMR = sm.f32(128, 64); NEGMR = sm.f32(128, 64)
        cp(IR, pr[0:64, 0:128], eng="act")
        cp(LFR, pr[0:64, 128:256], eng="act")
        scan(BR, LFR, ZER[0:64, :], 0.0, ALU.add, ALU.add)
        tt(AR, IR, BR, ALU.subtract)
        scan(CMR, AR, AR, -1e30, ALU.max, ALU.max)
        pc = bank()
        tr(pc[:, 0:64], AR, IDF[0:64, 0:64])
        tr(pc[:, 64:128], BR, IDF[0:64, 0:64])
        tr(pc[:, 128:192], CMR, IDF[0:64, 0:64])
        ABC = sm.f32(192)
        cp(ABC, pc[:, 0:192], eng="act")
        AC, BC, CMC = ABC[:, 0:64], ABC[:, 64:128], ABC[:, 128:192]
        TB = IR
        TB2 = LFR
        cp(TB, BR[:, 127:128].to_broadcast([64, 128]))
        cp(TB2, CMR[:, 127:128].to_broadcast([64, 128]))
        prp = bank()
        mm(prp[:, 0:64], TB, IDF[0:64, 0:64])
        mm(prp[:, 64:128], TB2, IDF[0:64, 0:64])
        REP = sm.f32(128)
        cp(REP, prp[:, 0:128], eng="act")
        BL, AMX = REP[:, 0:64], REP[:, 64:128]
        BINC = sm.f32(64)
        for h in range(4):
            scan(v3(BINC)[:, :, h], v3(BL)[:, :, h], ZER[:, 0:16], 0.0, ALU.add, ALU.add)
        REM = sm.f32(64)
        tt(REM, BINC, BL, ALU.subtract)
        tt(v3(REM), v3(BINC)[:, 15:16, :].to_broadcast([128, 16, 4]), v3(REM), ALU.subtract)
        VAL = sm.f32(64)
        tt(VAL, AMX, REM, ALU.add)
        MLOC = sm.f32(4)
        red(MLOC, VAL.rearrange("p (c h) -> p h c", h=4), ALU.max)
        TT_ = sm.f32(64)
        tt(v3(TT_), v3(REM), MLOC.unsqueeze(1).to_broadcast([128, 16, 4]), ALU.subtract)
        WEND = sm.f32(64)
        tt(WEND, AC, TT_, ALU.add)
        act(WEND, WEND, AF.Exp)
        tp.reset()
        WKA = [tp.bf(512).rearrange("p (h d) -> p h d", d=128) for _ in range(2)]
        VEX = [tp.bf(520).rearrange("p (h e) -> p h e", e=130) for _ in range(2)]
        for vv in VEX:
            memset(vv[:, :, 128:130], 1.0)
        K.reserved = {4, 5, 6, 7}
        CA = [PS[4 + h] for h in range(4)]
        for c in range(16):
            tsl = slice(c * 128, (c + 1) * 128)
            pk, pv_ = bank(), bank()
            for k in range(8):
                mm(pk[:, :], XNT[:, k, tsl], WS[3][:, k, :], k == 0, k == 7)
            for k in range(8):
                mm(pv_[:, :], XNT[:, k, tsl], WS[4][:, k, :], k == 0, k == 7)
            wk = WKA[c % 2]
            vx = VEX[c % 2]
            stt(wk, pk[:, :].rearrange("p (h d) -> p h d", d=128), KSC,
                v3(WEND)[:, c, :].unsqueeze(2).to_broadcast([128, 4, 128]), ALU.mult, ALU.mult)
            cp(vx[:, :, 0:128], pv_[:, :].rearrange("p (h e) -> p h e", e=128), eng="act")
            for h in range(4):
                mm(CA[h][:, 0:129], wk[:, h, :], vx[:, h, 0:129], c == 0, c == 15)
        for h in range(4):
            cp(SUMM[:, h * 129:(h + 1) * 129], CA[h][:, 0:129], eng="act")
        K.reserved = set()
        cp(SUMM[:, 516:520], MLOC)
        cp(SUMM[:, 520:524], v3(BINC)[:, 15, :])
        dma("sp", f"ccin{cci}", [(ccin[cci].ap(), SUMM)])
        csem = dsem(f"cc{cci}")

        def ccfn(h, cci=cci, csem=csem):
            return h.collective_compute("AllGather", ALU.bypass, replica_groups=[[0, 1, 2, 3], [4, 5, 6, 7]],
                                        ins=[ccin[cci].ap().opt()], outs=[ccout[cci].ap().opt()]).then_inc(csem)
        _cop = P.add("pool", ccfn, reads=[ccin[cci].ap()], writes=[ccout[cci].ap()], dma_slot=f"cc{cci}", n_dma=1)
        _cop.dma_cnt = 1
        P.dma_sems[f"cc{cci}"] = 1
        s2.reset()
        G = s2.f32(SUMW)
        CST_ = s2.f32(516).rearrange("p (h e) -> p h e", e=129)
        CBF = s2.bf(520).rearrange("p (h e) -> p h e", e=130)
        TMPC = s2.f32(129)
        MREP = sm.f32(4); HZ = sm.f32(8)
        BJ = sm.f32(4); MJ = sm.f32(4); T1_ = sm.f32(4); MN = sm.f32(4); S12 = sm.f32(8)
        memset(CST_, 0.0)
        memset(MREP, 0.0)
        memset(HZ, 0.0)
        for jr in range(3):
            dma("sp", "gload", [(G, ccout[cci].ap()[jr * 128:(jr + 1) * 128, :])])
            inc = FLG[:, jr:jr + 1]; neg = FLG[:, 4 + jr:5 + jr]; prv = FLG[:, 8 + jr:9 + jr]
            ts(BJ, G[:, 520:524], inc, None, ALU.mult)
            ts(MJ, G[:, 516:520], inc, neg, ALU.mult, ALU.add)
            tt(T1_, MREP, BJ, ALU.add)
            tt(MN, T1_, MJ, ALU.max)
            tt(S12[:, 0:4], T1_, MN, ALU.subtract)
            tt(S12[:, 4:8], MJ, MN, ALU.subtract)
            act(S12, S12, AF.Exp)
            for h in range(4):
                ts(TMPC, G[:, h * 129:(h + 1) * 129], S12[:, 4 + h:5 + h], None, ALU.mult)
                stt(CST_[:, h, :], CST_[:, h, :], S12[:, h:h + 1], TMPC, ALU.mult, ALU.add)
            cp(MREP, MN)
            stt(HZ, G[:, 524:532], prv, HZ, ALU.mult, ALU.add)
        cp(CBF[:, :, 0:129], CST_, eng="act")
        tp.reset()
        QT = tp.bf(512).rearrange("p (h d) -> p h d", d=128)
        KT = tp.bf(512).rearrange("p (h d) -> p h d", d=128)
        WK = tp.bf(512).rearrange("p (h d) -> p h d", d=128)
        VX = tp.bf(520).rearrange("p (h e) -> p h e", e=130)
        OG = tp.bf(512)
        RC = tp.f32(512, 64)
        DD = tp.bf(512).rearrange("p (h d) -> p h d", d=128)
        PP = tp.bf(512).rearrange("p (h d) -> p h d", d=128)
        QCS = tp.f32(516)
        ND = tp.f32(516)
        JNK = tp.bf(128)
        YB = tp.bf(512)
        YBT = tp.bf(512).rearrange("p (h d) -> p h d", d=128)
        DY = tp.bf(512).rearrange("p (c t) -> p c t", t=128)
        DEN = tp.f32(4); NDEN = tp.f32(4); RDEN = tp.f32(4); SSH = tp.f32(4); RSH = tp.f32(4)
        D0 = tp.f32(4); D1 = tp.f32(4); D2 = tp.f32(4)
        HZ3 = HZ.rearrange("p (c t) -> p c t", t=2)
        tt(D0, HZ3[:, :, 0], WC[:, :, 0], ALU.mult)
        tt(D2, HZ3[:, :, 1], WC[:, :, 1], ALU.mult)
        tt(D0, D0, D2, ALU.add)
        tt(D0, D0, BG01[:, :, 0], ALU.mult)
        tt(D1, HZ3[:, :, 1], WC[:, :, 0], ALU.mult)
        tt(D1, D1, BG01[:, :, 1], ALU.mult)
        memset(DY, 0.0)
        cp(DY[:, :, 0], D0)
        cp(DY[:, :, 1], D1)
        for half in range(2):
            po = bank()
            for cc in range(4):
                mm(po[:, :], DY[:, cc, :], WOUT[:, cc, half * 512:(half + 1) * 512], cc == 0, cc == 3)
            resid_add(0, half, po[:, :])
        MNEXT = sm.f32(64); MCUR = sm.f32(64); MX = sm.f32(64); DEC = sm.f32(64); WKB = sm.f32(64)
        MCOL = sm.f32(64); INTER = sm.f32(64); FL = sm.f32(64)
        for h in range(4):
            scan(v3(MNEXT)[:, :, h], v3(AMX)[:, :, h], v3(BL)[:, :, h], MREP[:, h:h + 1], ALU.max, ALU.add)
        cp(v3(MCUR)[:, 0, :], MREP)
        cp(v3(MCUR)[:, 1:16, :], v3(MNEXT)[:, 0:15, :])
        tt(MX, MCUR, AMX, ALU.max)
        tt(DEC, MCUR, MX, ALU.subtract)
        act(DEC, DEC, AF.Exp)
        tt(WKB, AC, MX, ALU.subtract)
        act(WKB, WKB, AF.Exp)
        tt(MCOL, CMC, MCUR, ALU.max)
        tt(INTER, MCUR, MCOL, ALU.subtract)
        act(INTER, INTER, AF.Exp)
        tt(FL, BC, MCOL, ALU.add)
        act(FL, FL, AF.Exp, scale=-1.0)
        T64 = AR[:, 0:64]
        MR = sm.f32(1, 64)
        tt(T64, MCUR[0:64, :], IDF[0:64, 0:64], ALU.mult)
        red(MR, T64, ALU.add)
        ts(NEGMR, CMR, MR, -1.0, ALU.max, ALU.mult)
        xb = Bump(O_GB, O_PH)
        QTb = [QT, xb.bf(512).rearrange("p (h d) -> p h d", d=128)]
        WKb = [WK, xb.bf(512).rearrange("p (h d) -> p h d", d=128)]
        VXb = [VX, xb.bf(520).rearrange("p (h e) -> p h e", e=130)]
        OGb = [OG, xb.bf(512)]
        PPb = [PP, xb.bf(512).rearrange("p (h d) -> p h d", d=128)]
        E1 = xb.f32(512)
        GHALF = xb.f32(512)
        ts(GHALF, GHEAD, 0.5, None, ALU.mult)
        for vv in VXb:
            memset(vv[:, :, 128:130], 1.0)
        ND3 = ND.rearrange("p (h e) -> p h e", e=129)

        def st_a(c):
            tsl = slice(c * 128, (c + 1) * 128)
            qt, wkb, vxb, og, pp = QTb[c % 2], WKb[c % 2], VXb[c % 2], OGb[c % 2], PPb[c % 2]
            pq = bank()
            for h in range(4):
                for k in range(8):
                    mm(pq[:, h * 128:(h + 1) * 128], WS[0][:, k, h * 128:(h + 1) * 128], XNT[:, k, tsl], k == 0, k == 7)
            cp(qt, pq[:, :].rearrange("p (h d) -> p h d", d=128), eng="act")
            pkk = bank()
            for h in range(4):
                for k in range(8):
                    mm(pkk[:, h * 128:(h + 1) * 128], WS[3][:, k, h * 128:(h + 1) * 128], XNT[:, k, tsl], k == 0, k == 7)
            cp(KT, pkk[:, :].rearrange("p (h d) -> p h d", d=128), eng="act")
            tt(RC.rearrange("p (h t) -> p h t", t=128), NEGMR.unsqueeze(1).to_broadcast([64, 4, 128]),
               IDF[0:64, 4 * c:4 * c + 4].unsqueeze(2).to_broadcast([64, 4, 128]), ALU.mult)
            pe_ = bank()
            mm(pe_[:, :], IDB, MNEG, True, False)
            mm(pe_[:, :], ONES[0:64, :], RC, False, True)
            ps_ = bank()
            for h in range(4):
                mm(ps_[:, h * 128:(h + 1) * 128], KT[:, h, :], qt[:, h, :])
            for h in range(4):
                act(DD[:, h, :], pe_[:, h * 128:(h + 1) * 128], AF.Exp, bias=AC[:, c * 4 + h:c * 4 + h + 1])
            stt(pp, ps_[:, :].rearrange("p (h d) -> p h d", d=128), KSC, DD, ALU.mult, ALU.mult)
            pkt = bank()
            for k in range(8):
                mm(pkt[:, :], XNT[:, k, tsl], WS[3][:, k, :], k == 0, k == 7)
            stt(wkb, pkt[:, :].rearrange("p (h d) -> p h d", d=128), KSC,
                v3(WKB)[:, c, :].unsqueeze(2).to_broadcast([128, 4, 128]), ALU.mult, ALU.mult)
            pv_ = bank()
            for k in range(8):
                mm(pv_[:, :], XNT[:, k, tsl], WS[4][:, k, :], k == 0, k == 7)
            cp(vxb[:, :, 0:128], pv_[:, :].rearrange("p (h e) -> p h e", e=128), eng="act")
            po_ = bank()
            for k in range(8):
                mm(po_[:, :], XNT[:, k, tsl], WS[1][:, k, :], k == 0, k == 7)
            act(E1, po_[:, :], AF.Tanh, scale=0.5)
            stt(og, E1, 1.0, GHALF, ALU.add, ALU.mult)

        def st_b1(c):
            qt, vxb, pp = QTb[c % 2], VXb[c % 2], PPb[c % 2]
            pO = [bank(), bank()]
            pQ = [bank(), bank()]
            for h in range(4):
                mm(pQ[h // 2][:, (h % 2) * 129:(h % 2) * 129 + 129], qt[:, h, :], CBF[:, h, 0:129])
            for h in range(4):
                mm(pO[h // 2][:, (h % 2) * 129:(h % 2) * 129 + 129], pp[:, h, :], vxb[:, h, 0:129])
            for h in range(4):
                act(QCS[:, h * 129:(h + 1) * 129], pQ[h // 2][:, (h % 2) * 129:(h % 2) * 129 + 129], AF.Identity,
                    scale=INTER[:, c * 4 + h:c * 4 + h + 1])
            for hh in range(2):
                tt(ND[:, hh * 258:(hh + 1) * 258], pO[hh][:, 0:258], QCS[:, hh * 258:(hh + 1) * 258], ALU.add)

        def st_b2a(c):
            og = OGb[c % 2]
            ts(NDEN, ND3[:, :, 128], -1.0, None, ALU.mult)
            tt(DEN, ND3[:, :, 128], NDEN, ALU.max)
            tt(DEN, DEN, v3(FL)[:, c, :], ALU.max)
            recip(RDEN, DEN)
            HV = ND3[:, :, 0:128]
            tt(HV, HV, RDEN.unsqueeze(2).to_broadcast([128, 4, 128]), ALU.mult)
            for h in range(4):
                stt(JNK, ND3[:, h, 0:128], 1.0, ND3[:, h, 0:128], ALU.mult, ALU.mult, accum=SSH[:, h:h + 1])
            rstd_op(RSH, SSH, 1.0 / 128, 4)
            tt(HV, HV, RSH.unsqueeze(2).to_broadcast([128, 4, 128]), ALU.mult)
            tt(YB.rearrange("p (h d) -> p h d", d=128), HV, og.rearrange("p (h d) -> p h d", d=128), ALU.mult)

        def st_b2b(c):
            wkb, vxb = WKb[c % 2], VXb[c % 2]
            pKV = [bank(), bank()]
            for h in range(4):
                mm(pKV[h // 2][:, (h % 2) * 129:(h % 2) * 129 + 129], wkb[:, h, :], vxb[:, h, 0:129])
            for h in range(4):
                stt(CST_[:, h, :], CST_[:, h, :], DEC[:, c * 4 + h:c * 4 + h + 1],
                    pKV[h // 2][:, (h % 2) * 129:(h % 2) * 129 + 129], ALU.mult, ALU.add)
            cp(CBF[:, :, 0:129], CST_, eng="act")

        def st_b2c(c):
            pT = bank()
            pTb = pT[:, :].bitcast(BF16)
            for h in range(4):
                tr(pTb[:, h * 128:(h + 1) * 128], YB[:, h * 128:(h + 1) * 128], IDB)
            cp(YBT, pTb[:, 0:512].rearrange("p (h d) -> p h d", d=128), eng="act")
            for half in range(2):
                po = bank()
                for h in range(4):
                    mm(po[:, :], YBT[:, h, :], WOUT[:, 4 + h, half * 512:(half + 1) * 512], h == 0, h == 3)
                resid_add(c, half, po[:, :])

        st_a(0)
        for c in range(16):
            st_b1(c)
            if c + 1 < 16:
                st_a(c + 1)
            st_b2b(c)
            st_b2a(c)
            st_b2c(c)
    K.even_mixer = even_mixer


    for kind, l in stages:
        if kind == "even":
            norm_to_xnt(gmix_d[l], 0)
            even_mixer(l, l // 2)
        elif kind == "odd":
            norm_to_xnt(gmix_d[l], 0)
            odd_mixer(l // 2)
        elif kind == "ffn":
            norm_to_xnt(gffn_d[l], 1)
            ffn(l)
    yout = y_d.rearrange("(t p) d -> p t d", p=128)
    if final_norm:
        g_b = GB[0]
        dma("sp", "gb0", [(g_b, gfin_d)])
        for t in range(NT):
            act(XS[t % 2], X[:, t, :], AF.Square, accum=SSQ[:, t:t + 1])
        rstd_op(RSTD, SSQ, 1.0 / D, 16)
        for t in range(NT):
            stt(X[:, t, :], X[:, t, :], RSTD[:, t:t + 1], g_b, ALU.mult, ALU.mult)
    for q4 in range(4):
        dma("sp", "yout", [(yout[:, q4 * 4:(q4 + 1) * 4, :], X[:, q4 * 4:(q4 + 1) * 4, :])])
    n_out = P.dma_sems["yout"]
    block = es.enter_context(nc.Block())

    def fin(h):
        h.wait_ge(sems_dma["yout"], n_out)
    P.emit(block, sems_eng, sems_dma, extra={"sp": fin})
    es.close()
    return nc


def _consts():
    s = np.arange(128)[:, None]
    t = np.arange(128)[None, :]
    ident = np.eye(128, dtype=np.float32)
    mst = (s <= t).astype(np.float32)
    ones = np.ones((128, 128), np.float32)
    c_f32 = np.concatenate([ident, mst, ones], axis=1)
    mneg = np.where(s <= t, 0.0, -30000.0).astype(np.float32)
    c_bf = np.concatenate([ident, np.tile(mneg, (1, 4))], axis=1)
    return np.ascontiguousarray(c_f32), np.ascontiguousarray(c_bf)


def make_in_maps(inp):
    f = lambda a: np.ascontiguousarray(np.asarray(a, dtype=np.float32))
    rep = lambda a: np.ascontiguousarray(np.broadcast_to(np.asarray(a, np.float32)[..., None, :], a.shape[:-1] + (128, a.shape[-1])))
    c_f32, c_bf = _consts()
    wc = np.asarray(inp["even_w_conv"], np.float32)
    wconvT = np.ascontiguousarray(wc.reshape(2, 3, 4, 128).transpose(0, 3, 2, 1).reshape(2, 128, 12))
    bs = np.asarray(inp["odd_b_s"], np.float32).reshape(2, 1024)
    shared = {
        "c_f32": c_f32, "c_bf": c_bf,
        "gmix_b": rep(inp["norm_mix"]), "gffn_b": rep(inp["norm_ffn"]), "gfin_b": rep(inp["norm_final"]),
        "bgate_b": rep(inp["even_b_gate"]), "wconvT": wconvT, "ghead_b": rep(inp["even_g_head"]),
        "gv_b": rep(inp["odd_g_v"]), "bs_b": rep(bs),
        "even_w_in": f(inp["even_w_in"]), "even_w_out": f(inp["even_w_out"]),
        "odd_w_in": f(inp["odd_w_in"]), "odd_w_s": f(inp["odd_w_s"]), "odd_w_out": f(inp["odd_w_out"]),
        "ffn_w_in": f(inp["ffn_w_in"]), "ffn_w_out": f(inp["ffn_w_out"]),
    }
    x = np.asarray(inp["x"], np.float32)
    maps = []
    for c in range(8):
        b, sg = c // 4, c % 4
        fl = np.zeros((128, 12), np.float32)
        for j in range(4):
            inc = 1.0 if j < sg else 0.0
            fl[:, j] = inc
            fl[:, 4 + j] = (inc - 1.0) * 1e30
            fl[:, 8 + j] = 1.0 if j == sg - 1 else 0.0
        m = dict(shared)
        m["x"] = np.ascontiguousarray(x[b, sg * TOK:(sg + 1) * TOK])
        m["flags"] = fl
        maps.append(m)
    return maps


def run_stages(inp, stages, final_norm):
    nc = build_program(stages, final_norm)
    maps = make_in_maps(inp)
    res = run_bass_kernel_spmd(nc, maps, core_ids=list(range(8)))
    out = np.empty((2, 8192, 1024), np.float32)
    for c in range(8):
        out[c // 4, (c % 4) * TOK:(c % 4 + 1) * TOK] = res.results[c]["y"]
    return out


def kernel(**inputs):
    stages = []
    for l in range(4):
        stages.append(("even" if l % 2 == 0 else "odd", l))
        stages.append(("ffn", l))
    return run_stages(inputs, stages, True)
```

```python
from contextlib import ExitStack
from concourse.bass_utils import run_bass_kernel_spmd
import numpy as np
import concourse.bass as bass
import concourse.mybir as mybir

F32 = mybir.dt.float32
BF16 = mybir.dt.bfloat16
ALU = mybir.AluOpType
AF = mybir.ActivationFunctionType
AX = mybir.AxisListType


import os
STRICT = bool(os.environ.get('BASS_STRICT'))


class _Op:
    __slots__ = ("eng", "idx", "fn", "waits", "signal", "dma_sem", "dma_cnt", "sigval", "isdma")

    def __init__(self, eng, idx, fn):
        self.eng = eng
        self.idx = idx
        self.fn = fn
        self.waits = []
        self.signal = False
        self.dma_sem = None
        self.dma_cnt = 0
        self.sigval = 0
        self.isdma = False


def _ap_range(ap):
    sp = str(ap.space)
    name = ap.tensor.name
    if "DRAM" in sp.upper() or sp.upper() not in ("SB", "PSUM"):
        return ("D:" + name, 0, 1)
    esz = mybir.dt.size(ap.dtype)
    pat = ap.ap
    pstride = pat[0][0]
    off = ap.offset % pstride if pstride > 0 else ap.offset
    ext = 0
    for st, cnt in pat[1:]:
        ext += abs(st) * (cnt - 1)
    lo = off * esz
    hi = (off + ext + 1) * esz
    if sp.upper() == "PSUM":
        lo, hi = (lo // 2048) * 2048, ((hi + 2047) // 2048) * 2048
    return (sp + ":" + name, lo, hi)


class Prog:
    ENGS = ("pe", "act", "dve", "pool", "sp")

    def __init__(self, nc):
        self.nc = nc
        self.ops = {e: [] for e in self.ENGS}
        self.segs = {}
        self.seen = {e: {f: -1 for f in self.ENGS} for e in self.ENGS}
        self.seen_sem = {e: {} for e in self.ENGS}
        self.dma_sems = {}
        self.extra_sems = []

    def _touch(self, key, lo, hi, op, is_write, deps):
        segs = self.segs.setdefault(key, [])
        new = []
        for s in segs:
            if s[1] <= lo or s[0] >= hi:
                new.append(s)
                continue
            cuts = [s[0]] + [c for c in (lo, hi) if s[0] < c < s[1]] + [s[1]]
            for a, b in zip(cuts[:-1], cuts[1:]):
                new.append([a, b, s[2], dict(s[3])])
        new.sort(key=lambda s: s[0])
        out = []
        cur = lo
        for s in new:
            if s[1] <= lo or s[0] >= hi:
                out.append(s)
                continue
            if s[0] > cur:
                out.append([cur, s[0], None, {}])
            out.append(s)
            cur = s[1]
        if cur < hi:
            out.append([cur, hi, None, {}])
        out.sort(key=lambda s: s[0])
        for s in out:
            if s[1] <= lo or s[0] >= hi:
                continue
            if s[2] is not None:
                deps.append((s[2], "raw" if not is_write else "waw"))
            if is_write:
                for r in s[3].values():
                    deps.append((r, "war"))
                s[2] = op
                s[3] = {}
            else:
                s[3][op.eng if op.dma_sem is None and not op.isdma else ("dma", id(op))] = op
        merged = []
        for s in out:
            if merged and merged[-1][1] == s[0] and merged[-1][2] is s[2] and merged[-1][3] == s[3]:
                merged[-1][1] = s[1]
            else:
                merged.append(s)
        self.segs[key] = merged

    def add(self, eng, fn, reads=(), writes=(), dma_slot=None, n_dma=1):
        op = _Op(eng, len(self.ops[eng]), fn)
        op.isdma = dma_slot is not None
        deps = []
        for ap in reads:
            k, lo, hi = ap if isinstance(ap, tuple) else _ap_range(ap)
            self._touch(k, lo, hi, op, False, deps)
        for ap in writes:
            k, lo, hi = ap if isinstance(ap, tuple) else _ap_range(ap)
            self._touch(k, lo, hi, op, True, deps)
        if dma_slot is not None:
            self.dma_sems[dma_slot] = self.dma_sems.get(dma_slot, 0) + 16 * n_dma
            op.dma_sem = dma_slot
            op.dma_cnt = self.dma_sems[dma_slot]
        best = {}
        for dop, kind in deps:
            if dop is op:
                continue
            if dop.dma_sem is not None:
                cur = self.seen_sem[eng].get(dop.dma_sem, 0)
                if dop.dma_cnt > cur:
                    self.seen_sem[eng][dop.dma_sem] = dop.dma_cnt
                    op.waits.append(("sem", dop.dma_sem, dop.dma_cnt))
                continue
            if dop.eng == eng and not op.isdma:
                if eng == "pe" or (kind != "raw" and not STRICT):
                    continue
            if dop.idx > best.get(dop.eng, -1):
                best[dop.eng] = dop.idx
        for f, idx in best.items():
            if idx > self.seen[eng][f]:
                self.seen[eng][f] = idx
                self.ops[f][idx].signal = True
                op.waits.append(("eng", f, idx))
        self.ops[eng].append(op)
        return op

    def emit(self, block, sems_eng, sems_dma, extra=None):
        nc = self.nc
        for e in self.ENGS:
            c = 0
            for op in self.ops[e]:
                if op.signal:
                    c += 1
                    op.sigval = c
        handles = {"pe": nc.tensor, "act": nc.scalar, "dve": nc.vector, "pool": nc.gpsimd, "sp": nc.sync}

        def body(e):
            def f(h):
                for op in self.ops[e]:
                    for w in op.waits:
                        if w[0] == "sem":
                            h.wait_ge(sems_dma[w[1]], w[2])
                        else:
                            h.wait_ge(sems_eng[w[1]], self.ops[w[1]][w[2]].sigval)
                    ins = op.fn(h)
                    if op.dma_sem is not None:
                        pass
                    elif op.signal:
                        ins.then_inc(sems_eng[e], 1)
                if extra and e in extra:
                    extra[e](h)
            return f
        block.tensor(body("pe"))
        block.scalar(body("act"))
        block.vector(body("dve"))
        block.gpsimd(body("pool"))
        block.sync(body("sp"))


NT = 16
TOK = 2048
D = 1024
EPS = 1e-6
KSC = 128 ** -0.5
SUMW = 532

O_X = 0
O_XNT = 65536
O_CST = 98304
O_GB = O_CST + 8192
O_XS = O_GB + 8192
O_PH = O_XS + 4096
PH_SIZE = 90112
ARENA = O_PH + PH_SIZE


class K:
    pass


def build_program(stages, final_norm=True):
    nc = bass.Bass("TRN2", target_bir_lowering=False)
    P = Prog(nc)
    dt_in = {}

    def din(name, shape):
        t = nc.dram_tensor(name, list(shape), F32, kind="ExternalInput")
        dt_in[name] = t
        return t.ap()

    x_d = din("x", [TOK, D])
    cf_d = din("c_f32", [128, 384])
    cb_d = din("c_bf", [128, 640])
    fl_d = din("flags", [128, 12])
    gmix_d = din("gmix_b", [4, 128, D])
    gffn_d = din("gffn_b", [4, 128, D])
    gfin_d = din("gfin_b", [128, D])
    bgate_d = din("bgate_b", [2, 128, 8])
    wconv_d = din("wconvT", [2, 128, 12])
    ghead_d = din("ghead_b", [2, 128, 512])
    gv_d = din("gv_b", [2, 128, D])
    bs_d = din("bs_b", [2, 128, D])
    ewin_d = din("even_w_in", [2, 1024, 3592])
    ewout_d = din("even_w_out", [2, 1024, 1024])
    owin_d = din("odd_w_in", [2, 1024, 2048])
    ows_d = din("odd_w_s", [2, 8, 128, 128])
    owout_d = din("odd_w_out", [2, 1024, 1024])
    fwin_d = din("ffn_w_in", [4, 1024, 5632])
    fwout_d = din("ffn_w_out", [4, 2816, 1024])
    y_d = nc.dram_tensor("y", [TOK, D], F32, kind="ExternalOutput").ap()
    ccin = [nc.dram_tensor(f"ccin{i}", [128, SUMW], F32) for i in range(2)]
    ccout = [nc.dram_tensor(f"ccout{i}", [512, SUMW], F32) for i in range(2)]

    es = ExitStack()
    A = es.enter_context(nc.sbuf_tensor("arena", [128, ARENA // 4], F32))
    PS = [es.enter_context(nc.psum_tensor(f"ps{i}", [128, 512], F32)) for i in range(8)]
    sems_eng = {e: es.enter_context(nc.semaphore("s_" + e)) for e in Prog.ENGS}
    sems_dma = {}

    def dsem(name):
        if name not in sems_dma:
            sems_dma[name] = es.enter_context(nc.semaphore("d_" + name))
        return sems_dma[name]

    def f32v(off, n, parts=128):
        return A[0:parts, off // 4: off // 4 + n]

    def bfv(off, n, parts=128):
        return A[0:parts, off // 4: off // 4 + (n + 1) // 2].bitcast(BF16)[:, 0:n]

    X = f32v(O_X, NT * D).rearrange("p (t d) -> p t d", d=D)
    XNT = bfv(O_XNT, 8 * TOK).rearrange("p (k t) -> p k t", t=TOK)
    c = O_CST
    IDF = f32v(c, 128); c += 512
    MST = f32v(c, 128); c += 512
    ONES = f32v(c, 128); c += 512
    IDB = bfv(c, 128); c += 256
    MNEG = bfv(c, 512); c += 1024
    FLG = f32v(c, 12); c += 48
    SSQ = f32v(c, 16); c += 64
    RSTD = f32v(c, 16); c += 64
    BGATE = f32v(c, 8); c += 32
    WCONV = f32v(c, 12); c += 48
    SM = f32v(c, 64); c += 256
    ZER = f32v(c, 128); c += 512
    GHEAD = f32v(c, 512); c += 2048
    NEGH = f32v(c, 16); c += 64
    assert c <= O_CST + 8192, c
    GB = [f32v(O_GB + i * 4096, D) for i in range(2)]
    XS = [bfv(O_XS + i * 2048, D) for i in range(2)]

    def mm(out, lhsT, rhs, start=True, stop=True):
        P.add("pe", lambda h, o=out, l=lhsT, r=rhs, s=start, e=stop: h.matmul(o, l, r, start=s, stop=e),
              reads=[lhsT, rhs], writes=[out])

    def tr(out, in_, ident):
        P.add("pe", lambda h, o=out, i=in_, d=ident: h.transpose(o, i, d), reads=[in_, ident], writes=[out])

    def act(out, in_, func, bias=None, scale=None, accum=None):
        rd = [in_] + [v for v in (bias, scale) if v is not None and not isinstance(v, (int, float))]
        wr = [out] + ([accum] if accum is not None else [])
        kw = {}
        if bias is not None:
            kw["bias"] = bias
        if scale is not None:
            kw["scale"] = scale
        if accum is not None:
            kw["accum_out"] = accum
        P.add("act", lambda h, o=out, i=in_, f=func, k=kw: h.activation(out=o, in_=i, func=f, **k), reads=rd, writes=wr)

    def tt(out, in0, in1, op, eng="dve"):
        P.add(eng, lambda h, o=out, a=in0, b=in1, p=op: h.tensor_tensor(out=o, in0=a, in1=b, op=p),
              reads=[in0, in1], writes=[out])

    def ts(out, in0, s1, s2, op0, op1=None, eng="dve"):
        rd = [in0] + [v for v in (s1, s2) if v is not None and not isinstance(v, (int, float))]
        if op1 is None:
            P.add(eng, lambda h, o=out, a=in0, x=s1, p=op0: h.tensor_scalar(out=o, in0=a, scalar1=x, scalar2=None, op0=p),
                  reads=rd, writes=[out])
        else:
            P.add(eng, lambda h, o=out, a=in0, x=s1, y=s2, p=op0, q=op1: h.tensor_scalar(out=o, in0=a, scalar1=x, scalar2=y, op0=p, op1=q),
                  reads=rd, writes=[out])

    def stt(out, in0, scalar, in1, op0, op1, accum=None):
        rd = [in0, in1] + ([scalar] if not isinstance(scalar, (int, float)) else [])
        wr = [out] + ([accum] if accum is not None else [])
        if accum is None:
            P.add("dve", lambda h, o=out, a=in0, s=scalar, b=in1, p=op0, q=op1: h.scalar_tensor_tensor(out=o, in0=a, scalar=s, in1=b, op0=p, op1=q),
                  reads=rd, writes=wr)
        else:
            P.add("dve", lambda h, o=out, a=in0, s=scalar, b=in1, p=op0, q=op1, ac=accum: h.scalar_tensor_tensor(out=o, in0=a, scalar=s, in1=b, op0=p, op1=q, accum_out=ac),
                  reads=rd, writes=wr)

    def cp(out, in_, eng="dve"):
        if eng == "act":
            act(out, in_, AF.Copy)
        else:
            P.add(eng, lambda h, o=out, i=in_: h.tensor_copy(out=o, in_=i), reads=[in_], writes=[out])

    def scan(out, d0, d1, init, op0, op1):
        rd = [d0, d1] + ([init] if not isinstance(init, (int, float)) else [])
        P.add("dve", lambda h, o=out, a=d0, b=d1, i=init, p=op0, q=op1: h.tensor_tensor_scan(out=o, data0=a, data1=b, initial=i, op0=p, op1=q),
              reads=rd, writes=[out])

    def red(out, in_, op):
        P.add("dve", lambda h, o=out, i=in_, p=op: h.tensor_reduce(out=o, in_=i, axis=AX.X, op=p), reads=[in_], writes=[out])

    def recip(out, in_):
        P.add("dve", lambda h, o=out, i=in_: h.reciprocal(out=o, in_=i), reads=[in_], writes=[out])

    def memset(out, val, eng="pool"):
        P.add(eng, lambda h, o=out, v=val: h.memset(o, v), writes=[out])

    def dma(queue, slot, pairs, slot_ap=None, extra_reads=()):
        sem = dsem(slot)
        def fn(h, pairs=pairs, sem=sem):
            r = None
            for o, i in pairs:
                r = h.dma_start(out=o, in_=i).then_inc(sem, 16)
            return r
        wr = [slot_ap] if slot_ap is not None else [o for o, _ in pairs]
        P.add(queue, fn, reads=[i for _, i in pairs] + list(extra_reads), writes=wr, dma_slot=slot, n_dma=len(pairs))

    def rstd_op(out, ss, scale, n):
        ts(out, ss, scale, EPS, ALU.mult, ALU.add)
        P.add("pool", lambda h, o=out, nh=NEGH[:, 0:n]: h.tensor_tensor(out=o, in0=o, in1=nh, op=ALU.pow),
              reads=[out, NEGH[:, 0:n]], writes=[out])

    _bank = [0]

    def bank():
        b = PS[_bank[0] % 8]
        _bank[0] += 1
        return b

    dma("sp", "cst", [(A[:, O_CST // 4: O_CST // 4 + 384], cf_d), (FLG, fl_d)], slot_ap=A[:, O_CST // 4: (O_CST + 8192) // 4])
    dma("pool", "cstb", [(bfv(O_CST + 1536, 640), cb_d)], slot_ap=bfv(O_CST + 1536, 640))
    memset(ZER, 0.0)
    memset(NEGH, -0.5)
    xin = x_d.rearrange("(t p) d -> p t d", p=128)
    for q4 in range(4):
        dma("sp", f"xin{q4}", [(X[:, q4 * 4:(q4 + 1) * 4, :], xin[:, q4 * 4:(q4 + 1) * 4, :])])

    def norm_to_xnt(g_src, gslot):
        g_b = GB[gslot]
        dma("sp", f"gb{gslot}", [(g_b, g_src)])
        for t in range(NT):
            act(XS[t % 2], X[:, t, :], AF.Square, accum=SSQ[:, t:t + 1])
        rstd_op(RSTD, SSQ, 1.0 / D, 16)
        for t in range(NT):
            xs = XS[t % 2]
            stt(xs, X[:, t, :], RSTD[:, t:t + 1], g_b, ALU.mult, ALU.mult)
            pb = bank()
            pbb = pb[:, :].bitcast(BF16)
            for k in range(8):
                tr(pbb[:, k * 128:(k + 1) * 128], xs[:, k * 128:(k + 1) * 128], IDB)
            cp(XNT[:, :, t * 128:(t + 1) * 128], pbb.rearrange("p (k t) -> p k t", t=128), eng="act")

    def resid_add(t, half, ps):
        xv = X[:, t, half * 512:(half + 1) * 512]
        tt(xv, ps, xv, ALU.add)

    def ffn(l):
        groups = [(0, 6), (6, 6), (12, 5), (17, 5)]
        WI = [bfv(O_PH + s * 36864, 8 * 2 * 768).rearrange("p (k u n) -> p k u n", u=2, n=768) for s in range(2)]
        WO = [bfv(O_PH + s * 36864 + 24576, 6 * 1024).rearrange("p (j d) -> p j d", d=1024) for s in range(2)]
        ACTB = [bfv(O_PH + 73728 + s * 6144, 6 * 512).rearrange("p (j t) -> p j t", t=512) for s in range(2)]
        SG = [f32v(O_PH + 86016 + s * 2048, 512) for s in range(2)]
        it = 0
        for gi, (c0, n) in enumerate(groups):
            s = gi % 2
            slot_ap = bfv(O_PH + s * 36864, 18432)
            win = fwin_d[l].rearrange("(k p) n -> p k n", p=128)
            dma("pool", f"ffw{s}", [
                (WI[s][:, :, 0, 0:n * 128], win[:, :, c0 * 128:(c0 + n) * 128]),
                (WI[s][:, :, 1, 0:n * 128], win[:, :, 2816 + c0 * 128:2816 + (c0 + n) * 128]),
                (WO[s][:, 0:n, :], fwout_d[l][c0 * 128:(c0 + n) * 128, :].rearrange("(j p) d -> p j d", p=128)),
            ], slot_ap=slot_ap)
            for b in range(4):
                ab = ACTB[it % 2]
                it += 1
                for j in range(n):
                    pg = bank()
                    pu = bank()
                    for k in range(8):
                        mm(pg[:, :], WI[s][:, k, 0, j * 128:(j + 1) * 128], XNT[:, k, b * 512:(b + 1) * 512], k == 0, k == 7)
                    for k in range(8):
                        mm(pu[:, :], WI[s][:, k, 1, j * 128:(j + 1) * 128], XNT[:, k, b * 512:(b + 1) * 512], k == 0, k == 7)
                    sg = SG[j % 2]
                    act(sg, pg[:, :], AF.Silu)
                    tt(ab[:, j, :], pu[:, :], sg, ALU.mult)
                for i in range(4):
                    t = b * 4 + i
                    for half in range(2):
                        po = bank()
                        for j in range(n):
                            mm(po[:, :], ab[:, j, i * 128:(i + 1) * 128], WO[s][:, j, half * 512:(half + 1) * 512], j == 0, j == n - 1)
                        resid_add(t, half, po[:, :])

    def odd_mixer(j):
        o = O_PH
        WU = bfv(o, 8 * 1024).rearrange("p (k n) -> p k n", n=1024); o += 16384
        WV = bfv(o, 8 * 1024).rearrange("p (k n) -> p k n", n=1024); o += 16384
        WOUT = bfv(o, 8 * 1024).rearrange("p (k n) -> p k n", n=1024); o += 16384
        WST = bfv(o, 8 * 128).rearrange("p (g t) -> p g t", t=128); o += 2048
        BSB = f32v(o, 1024); o += 4096
        GVB = f32v(o, 1024); o += 4096
        UT = bfv(o, 8 * 512).rearrange("p (g t) -> p g t", t=512); o += 8192
        VG = f32v(o, 1024); o += 4096
        VN = bfv(o, 1024); o += 2048
        T1 = f32v(o, 1024); o += 4096
        GT = bfv(o, 1024).rearrange("p (g t) -> p g t", t=128); o += 2048
        WSS = f32v(o, 1024).rearrange("p (g s) -> p g s", s=128); o += 4096
        JNK = bfv(o, 1024); o += 2048
        VNB = bfv(o, 1024); o += 2048
        assert o <= O_PH + PH_SIZE
        UT2 = bfv(O_GB, 8 * 512).rearrange("p (g t) -> p g t", t=512)
        win = owin_d[j].rearrange("(k p) n -> p k n", p=128)
        dma("pool", "owu", [(WU, win[:, :, 0:1024])])
        dma("pool", "owv", [(WV, win[:, :, 1024:2048])])
        dma("pool", "owo", [(WOUT, owout_d[j].rearrange("(k p) n -> p k n", p=128))])
        dma("sp", "ows", [(WSS, ows_d[j].rearrange("g t s -> t g s"))])
        dma("sp", "obs", [(BSB, bs_d[j])])
        dma("sp", "ogv", [(GVB, gv_d[j])])
        for g in range(8):
            pb = bank()
            tr(pb[:, 0:128], WSS[:, g, :], IDF)
            tt(WST[:, g, :], pb[:, 0:128], MST, ALU.mult)
        VN2 = [VN, VNB]
        UTB = [UT, UT2]

        def stage_a(t):
            vn = VN2[t % 2]
            pv = [bank(), bank()]
            for half in range(2):
                for k in range(8):
                    mm(pv[half][:, :], XNT[:, k, t * 128:(t + 1) * 128], WV[:, k, half * 512:(half + 1) * 512], k == 0, k == 7)
            for half in range(2):
                act(VG[:, half * 512:(half + 1) * 512], pv[half][:, :], AF.Gelu_apprx_tanh)
            ss = SM[:, 0:1]
            stt(JNK, VG, 1.0, VG, ALU.mult, ALU.mult, accum=ss)
            rs = SM[:, 1:2]
            rstd_op(rs, ss, 1.0 / 1024, 1)
            stt(vn, VG, rs, GVB, ALU.mult, ALU.mult)

        def stage_b1(t):
            vn = VN2[t % 2]
            b, i = t // 4, t % 4
            ut = UTB[b % 2]
            pg = [bank(), bank()]
            for g in range(8):
                mm(pg[g // 4][:, (g % 4) * 128:(g % 4 + 1) * 128], vn[:, g * 128:(g + 1) * 128], WST[:, g, :])
            for hf in range(2):
                tt(T1[:, hf * 512:(hf + 1) * 512], pg[hf][:, :], BSB[:, hf * 512:(hf + 1) * 512], ALU.add)
            tt(GT, T1.rearrange("p (g t) -> p g t", t=128), ut[:, :, i * 128:(i + 1) * 128], ALU.mult)

        def stage_b2(t):
            for half in range(2):
                po = bank()
                for g in range(8):
                    mm(po[:, :], GT[:, g, :], WOUT[:, g, half * 512:(half + 1) * 512], g == 0, g == 7)
                resid_add(t, half, po[:, :])

        def ublock(b):
            for fc in range(8):
                pb = bank()
                for k in range(8):
                    mm(pb[:, :], WU[:, k, fc * 128:(fc + 1) * 128], XNT[:, k, b * 512:(b + 1) * 512], k == 0, k == 7)
                act(UTB[b % 2][:, fc, :], pb[:, :], AF.Gelu_apprx_tanh)

        ublock(0)
        stage_a(0)
        for t in range(16):
            if t + 1 < 16:
                if (t + 1) % 4 == 0:
                    ublock((t + 1) // 4)
                stage_a(t + 1)
            stage_b1(t)
            stage_b2(t)

    K.norm_to_xnt = norm_to_xnt
    K.ffn = ffn
    K.odd_mixer = odd_mixer
    class Bump:
        def __init__(self, base, limit):
            self.base, self.limit, self.cur = base, limit, base

        def reset(self):
            self.cur = self.base

        def _take(self, nbytes):
            o = self.cur
            self.cur += (nbytes + 31) // 32 * 32
            assert self.cur <= self.limit, (self.cur, self.limit)
            return o

        def f32(self, n, parts=128):
            return f32v(self._take(n * 4), n, parts)

        def bf(self, n, parts=128):
            return bfv(self._take(n * 2), n, parts)

    K.reserved = set()
    _obank = bank

    def bank():
        while True:
            i = _bank[0] % 8
            _bank[0] += 1
            if i not in K.reserved:
                return PS[i]

    def even_mixer(l, j):
        cci = j
        ws_off = [O_PH + i * 8192 for i in range(5)]
        WS = [bfv(ws_off[i], 8 * 512).rearrange("p (k n) -> p k n", n=512) for i in range(5)]
        WOUT = bfv(O_PH + 40960, 8 * 1024).rearrange("p (k n) -> p k n", n=1024)
        sm = Bump(O_PH + 57344, O_PH + 71680)
        tp = Bump(O_PH + 71680, O_PH + PH_SIZE)
        s2 = Bump(ws_off[2], ws_off[2] + 8192)
        win = ewin_d[j].rearrange("(k p) n -> p k n", p=128)

        def loadw(slot, col0):
            dma("pool", f"ew{slot}", [(WS[slot], win[:, :, col0:col0 + 512])])
        loadw(0, 0); loadw(1, 512); loadw(2, 1024)
        dma("pool", "ewo", [(WOUT, ewout_d[j].rearrange("(k p) n -> p k n", p=128))])
        loadw(3, 2048); loadw(4, 2560)
        WG = sm.bf(64).rearrange("p (k n) -> p k n", n=8)
        dma("pool", "ewg", [(WG, win[:, :, 3584:3592])])
        dma("sp", "ebg", [(BGATE, bgate_d[j])])
        dma("sp", "ewc", [(WCONV, wconv_d[j])])
        dma("sp", "egh", [(GHEAD, ghead_d[j])])
        v3 = lambda ap: ap.rearrange("p (c h) -> p c h", h=4)
        ZH = sm.f32(8).rearrange("p (c t) -> p c t", t=2)
        Z01 = sm.f32(8).rearrange("p (c t) -> p c t", t=2)
        BG01 = sm.f32(8).rearrange("p (c t) -> p c t", t=2)
        SUMM = sm.f32(SUMW)
        WC = WCONV.rearrange("p (c j) -> p c j", j=3)
        tp.reset()
        YAB = tp.bf(4 * 512).rearrange("p (c t) -> p c t", t=512)
        XA = tp.f32(512)
        Z = [tp.f32(514) for _ in range(2)]
        ACC = tp.f32(512)
        memset(ZH, 0.0)
        it = 0
        for b in range(4):
            for cc in range(4):
                pbg, pcg, pxa = bank(), bank(), bank()
                for pp, sl in ((pbg, 0), (pcg, 1), (pxa, 2)):
                    for k in range(8):
                        mm(pp[:, :], WS[sl][:, k, cc * 128:(cc + 1) * 128], XNT[:, k, b * 512:(b + 1) * 512], k == 0, k == 7)
                z = Z[it % 2]
                it += 1
                cp(XA, pxa[:, :], eng="act")
                cp(z[:, 0:2], ZH[:, cc, :], eng="pool")
                tt(z[:, 2:514], pcg[:, :], XA, ALU.mult)
                ts(ACC, z[:, 2:514], WC[:, cc, 2:3], None, ALU.mult)
                stt(ACC, z[:, 1:513], WC[:, cc, 1:2], ACC, ALU.mult, ALU.add)
                stt(ACC, z[:, 0:512], WC[:, cc, 0:1], ACC, ALU.mult, ALU.add)
                tt(YAB[:, cc, :], pbg[:, :], ACC, ALU.mult)
                cp(ZH[:, cc, :], z[:, 512:514], eng="pool")
                if b == 0:
                    cp(Z01[:, cc, :], z[:, 2:4], eng="pool")
                    cp(BG01[:, cc, :], pbg[:, 0:2], eng="act")
                if b == 3:
                    cp(SUMM[:, 524 + cc * 2:526 + cc * 2], z[:, 512:514], eng="pool")
            for i in range(4):
                t = b * 4 + i
                for half in range(2):
                    po = bank()
                    for cc in range(4):
                        mm(po[:, :], YAB[:, cc, i * 128:(i + 1) * 128], WOUT[:, cc, half * 512:(half + 1) * 512], cc == 0, cc == 3)
                    resid_add(t, half, po[:, :])
        loadw(0, 1536)
        loadw(1, 3072)
        GC = sm.f32(128).rearrange("p (t n) -> p t n", n=8)
        pgt = bank()
        for t in range(16):
            for k in range(8):
                mm(pgt[:, t * 8:(t + 1) * 8], XNT[:, k, t * 128:(t + 1) * 128], WG[:, k, :], k == 0, k == 7)
        tt(GC, pgt[:, 0:128].rearrange("p (t n) -> p t n", n=8), BGATE.unsqueeze(1).to_broadcast([128, 16, 8]), ALU.add)
        IC = sm.f32(64); LFC = sm.f32(64); EX = sm.f32(64)
        cp(v3(IC), GC[:, :, 0:4])
        act(v3(EX), GC[:, :, 4:8], AF.Exp, scale=-1.0)
        act(EX, EX, AF.Ln, bias=1.0)
        ts(LFC, EX, -1.0, None, ALU.mult)
        pr = bank()
        tr(pr[0:64, 0:128], IC, IDF)
        tr(pr[0:64, 128:256], LFC, IDF)
        IR = sm.f32(128, 64); LFR = sm.f32(128, 64); BR = sm.f32(128, 64); AR = sm.f32(128, 64)
        CMR = sm.f32(128, 64); NEGMR = sm.f32(128, 64)
        cp(IR, pr[0:64, 0:128], eng="act")
        cp(LFR, pr[0:64, 128:256], eng="act")
        scan(BR, LFR, ZER[0:64, :], 0.0, ALU.add, ALU.add)
        tt(AR, IR, BR, ALU.subtract)
        scan(CMR, AR, AR, -1e30, ALU.max, ALU.max)
        pc = bank()
        tr(pc[:, 0:64], AR, IDF[0:64, 0:64])
        tr(pc[:, 64:128], BR, IDF[0:64, 0:64])
        tr(pc[:, 128:192], CMR, IDF[0:64, 0:64])
        ABC = sm.f32(192)
        cp(ABC, pc[:, 0:192], eng="act")
        AC, BC, CMC = ABC[:, 0:64], ABC[:, 64:128], ABC[:, 128:192]
        TB = IR
        TB2 = LFR
        cp(TB, BR[:, 127:128].to_broadcast([64, 128]))
        cp(TB2, CMR[:, 127:128].to_broadcast([64, 128]))
        prp = bank()
        mm(prp[:, 0:64], TB, IDF[0:64, 0:64])
        mm(prp[:, 64:128], TB2, IDF[0:64, 0:64])
        REP = sm.f32(128)
        cp(REP, prp[:, 0:128], eng="act")
        BL, AMX = REP[:, 0:64], REP[:, 64:128]
        BINC = sm.f32(64)
        for h in range(4):
            scan(v3(BINC)[:, :, h], v3(BL)[:, :, h], ZER[:, 0:16], 0.0, ALU.add, ALU.add)
        REM = sm.f32(64)
        tt(REM, BINC, BL, ALU.subtract)
        tt(v3(REM), v3(BINC)[:, 15:16, :].to_broadcast([128, 16, 4]), v3(REM), ALU.subtract)
        VAL = sm.f32(64)
        tt(VAL, AMX, REM, ALU.add)
        MLOC = sm.f32(4)
        red(MLOC, VAL.rearrange("p (c h) -> p h c", h=4), ALU.max)
        TT_ = sm.f32(64)
        tt(v3(TT_), v3(REM), MLOC.unsqueeze(1).to_broadcast([128, 16, 4]), ALU.subtract)
        WEND = sm.f32(64)
        tt(WEND, AC, TT_, ALU.add)
        act(WEND, WEND, AF.Exp)
        tp.reset()
        WKA = [tp.bf(512).rearrange("p (h d) -> p h d", d=128) for _ in range(2)]
        VEX = [tp.bf(520).rearrange("p (h e) -> p h e", e=130) for _ in range(2)]
        for vv in VEX:
            memset(vv[:, :, 128:130], 1.0)
        K.reserved = {4, 5, 6, 7}
        CA = [PS[4 + h] for h in range(4)]
        for c in range(16):
            tsl = slice(c * 128, (c + 1) * 128)
            pk, pv_ = bank(), bank()
            for k in range(8):
                mm(pk[:, :], XNT[:, k, tsl], WS[3][:, k, :], k == 0, k == 7)
            for k in range(8):
                mm(pv_[:, :], XNT[:, k, tsl], WS[4][:, k, :], k == 0, k == 7)
            wk = WKA[c % 2]
            vx = VEX[c % 2]
            stt(wk, pk[:, :].rearrange("p (h d) -> p h d", d=128), KSC,
                v3(WEND)[:, c, :].unsqueeze(2).to_broadcast([128, 4, 128]), ALU.mult, ALU.mult)
            cp(vx[:, :, 0:128], pv_[:, :].rearrange("p (h e) -> p h e", e=128), eng="act")
            for h in range(4):
                mm(CA[h][:, 0:129], wk[:, h, :], vx[:, h, 0:129], c == 0, c == 15)
        for h in range(4):
            cp(SUMM[:, h * 129:(h + 1) * 129], CA[h][:, 0:129], eng="act")
        K.reserved = set()
        cp(SUMM[:, 516:520], MLOC)
        cp(SUMM[:, 520:524], v3(BINC)[:, 15, :])
        dma("sp", f"ccin{cci}", [(ccin[cci].ap(), SUMM)])
        csem = dsem(f"cc{cci}")

        def ccfn(h, cci=cci, csem=csem):
            return h.collective_compute("AllGather", ALU.bypass, replica_groups=[[0, 1, 2, 3], [4, 5, 6, 7]],
                                        ins=[ccin[cci].ap().opt()], outs=[ccout[cci].ap().opt()]).then_inc(csem)
        _cop = P.add("pool", ccfn, reads=[ccin[cci].ap()], writes=[ccout[cci].ap()], dma_slot=f"cc{cci}", n_dma=1)
        _cop.dma_cnt = 1
        P.dma_sems[f"cc{cci}"] = 1
        s2.reset()
        G = s2.f32(SUMW)
        CST_ = s2.f32(516).rearrange("p (h e) -> p h e", e=129)
        CBF = s2.bf(520).rearrange("p (h e) -> p h e", e=130)
        TMPC = s2.f32(129)
        MREP = sm.f32(4); HZ = sm.f32(8)
        BJ = sm.f32(4); MJ = sm.f32(4); T1_ = sm.f32(4); MN = sm.f32(4); S12 = sm.f32(8)
        memset(CST_, 0.0)
        memset(MREP, 0.0)
        memset(HZ, 0.0)
        for jr in range(3):
            dma("sp", "gload", [(G, ccout[cci].ap()[jr * 128:(jr + 1) * 128, :])])
            inc = FLG[:, jr:jr + 1]; neg = FLG[:, 4 + jr:5 + jr]; prv = FLG[:, 8 + jr:9 + jr]
            ts(BJ, G[:, 520:524], inc, None, ALU.mult)
            ts(MJ, G[:, 516:520], inc, neg, ALU.mult, ALU.add)
            tt(T1_, MREP, BJ, ALU.add)
            tt(MN, T1_, MJ, ALU.max)
            tt(S12[:, 0:4], T1_, MN, ALU.subtract)
            tt(S12[:, 4:8], MJ, MN, ALU.subtract)
            act(S12, S12, AF.Exp)
            for h in range(4):
                ts(TMPC, G[:, h * 129:(h + 1) * 129], S12[:, 4 + h:5 + h], None, ALU.mult)
                stt(CST_[:, h, :], CST_[:, h, :], S12[:, h:h + 1], TMPC, ALU.mult, ALU.add)
            cp(MREP, MN)
            stt(HZ, G[:, 524:532], prv, HZ, ALU.mult, ALU.add)
        cp(CBF[:, :, 0:129], CST_, eng="act")
        tp.reset()
        QT = tp.bf(512).rearrange("p (h d) -> p h d", d=128)
        KT = tp.bf(512).rearrange("p (h d) -> p h d", d=128)
        WK = tp.bf(512).rearrange("p (h d) -> p h d", d=128)
        VX = tp.bf(520).rearrange("p (h e) -> p h e", e=130)
        OG = tp.bf(512)
        RC = tp.f32(512, 64)
        DD = tp.bf(512).rearrange("p (h d) -> p h d", d=128)
        PP = tp.bf(512).rearrange("p (h d) -> p h d", d=128)
        QCS = tp.f32(516)
        ND = tp.f32(516)
        JNK = tp.bf(128)
        YB = tp.bf(512)
        YBT = tp.bf(512).rearrange("p (h d) -> p h d", d=128)
        DY = tp.bf(512).rearrange("p (c t) -> p c t", t=128)
        DEN = tp.f32(4); NDEN = tp.f32(4); RDEN = tp.f32(4); SSH = tp.f32(4); RSH = tp.f32(4)
        D0 = tp.f32(4); D1 = tp.f32(4); D2 = tp.f32(4)
        HZ3 = HZ.rearrange("p (c t) -> p c t", t=2)
        tt(D0, HZ3[:, :, 0], WC[:, :, 0], ALU.mult)
        tt(D2, HZ3[:, :, 1], WC[:, :, 1], ALU.mult)
        tt(D0, D0, D2, ALU.add)
        tt(D0, D0, BG01[:, :, 0], ALU.mult)
        tt(D1, HZ3[:, :, 1], WC[:, :, 0], ALU.mult)
        tt(D1, D1, BG01[:, :, 1], ALU.mult)
        memset(DY, 0.0)
        cp(DY[:, :, 0], D0)
        cp(DY[:, :, 1], D1)
        for half in range(2):
            po = bank()
            for cc in range(4):
                mm(po[:, :], DY[:, cc, :], WOUT[:, cc, half * 512:(half + 1) * 512], cc == 0, cc == 3)
            resid_add(0, half, po[:, :])
        MNEXT = sm.f32(64); MCUR = sm.f32(64); MX = sm.f32(64); DEC = sm.f32(64); WKB = sm.f32(64)
        MCOL = sm.f32(64); INTER = sm.f32(64); FL = sm.f32(64)
        for h in range(4):
            scan(v3(MNEXT)[:, :, h], v3(AMX)[:, :, h], v3(BL)[:, :, h], MREP[:, h:h + 1], ALU.max, ALU.add)
        cp(v3(MCUR)[:, 0, :], MREP)
        cp(v3(MCUR)[:, 1:16, :], v3(MNEXT)[:, 0:15, :])
        tt(MX, MCUR, AMX, ALU.max)
        tt(DEC, MCUR, MX, ALU.subtract)
        act(DEC, DEC, AF.Exp)
        tt(WKB, AC, MX, ALU.subtract)
        act(WKB, WKB, AF.Exp)
        tt(MCOL, CMC, MCUR, ALU.max)
        tt(INTER, MCUR, MCOL, ALU.subtract)
        act(INTER, INTER, AF.Exp)
        tt(FL, BC, MCOL, ALU.add)
        act(FL, FL, AF.Exp, scale=-1.0)
        T64 = AR[:, 0:64]
        MR = sm.f32(1, 64)
        tt(T64, MCUR[0:64, :], IDF[0:64, 0:64], ALU.mult)
        red(MR, T64, ALU.add)
        ts(NEGMR, CMR, MR, -1.0, ALU.max, ALU.mult)
        xb = Bump(O_GB, O_PH)
        QTb = [QT, xb.bf(512).rearrange("p (h d) -> p h d", d=128)]
        WKb = [WK, xb.bf(512).rearrange("p (h d) -> p h d", d=128)]
        VXb = [VX, xb.bf(520).rearrange("p (h e) -> p h e", e=130)]
        OGb = [OG, xb.bf(512)]
        PPb = [PP, xb.bf(512).rearrange("p (h d) -> p h d", d=128)]
        E1 = xb.f32(512)
        GHALF = xb.f32(512)
        ts(GHALF, GHEAD, 0.5, None, ALU.mult)
        for vv in VXb:
            memset(vv[:, :, 128:130], 1.0)
        ND3 = ND.rearrange("p (h e) -> p h e", e=129)

        def st_a(c):
            tsl = slice(c * 128, (c + 1) * 128)
            qt, wkb, vxb, og, pp = QTb[c % 2], WKb[c % 2], VXb[c % 2], OGb[c % 2], PPb[c % 2]
            pq = bank()
            for h in range(4):
                for k in range(8):
                    mm(pq[:, h * 128:(h + 1) * 128], WS[0][:, k, h * 128:(h + 1) * 128], XNT[:, k, tsl], k == 0, k == 7)
            cp(qt, pq[:, :].rearrange("p (h d) -> p h d", d=128), eng="act")
            pkk = bank()
            for h in range(4):
                for k in range(8):
                    mm(pkk[:, h * 128:(h + 1) * 128], WS[3][:, k, h * 128:(h + 1) * 128], XNT[:, k, tsl], k == 0, k == 7)
            cp(KT, pkk[:, :].rearrange("p (h d) -> p h d", d=128), eng="act")
            tt(RC.rearrange("p (h t) -> p h t", t=128), NEGMR.unsqueeze(1).to_broadcast([64, 4, 128]),
               IDF[0:64, 4 * c:4 * c + 4].unsqueeze(2).to_broadcast([64, 4, 128]), ALU.mult)
            pe_ = bank()
            mm(pe_[:, :], IDB, MNEG, True, False)
            mm(pe_[:, :], ONES[0:64, :], RC, False, True)
            ps_ = bank()
            for h in range(4):
                mm(ps_[:, h * 128:(h + 1) * 128], KT[:, h, :], qt[:, h, :])
            for h in range(4):
                act(DD[:, h, :], pe_[:, h * 128:(h + 1) * 128], AF.Exp, bias=AC[:, c * 4 + h:c * 4 + h + 1])
            stt(pp, ps_[:, :].rearrange("p (h d) -> p h d", d=128), KSC, DD, ALU.mult, ALU.mult)
            pkt = bank()
            for k in range(8):
                mm(pkt[:, :], XNT[:, k, tsl], WS[3][:, k, :], k == 0, k == 7)
            stt(wkb, pkt[:, :].rearrange("p (h d) -> p h d", d=128), KSC,
                v3(WKB)[:, c, :].unsqueeze(2).to_broadcast([128, 4, 128]), ALU.mult, ALU.mult)
            pv_ = bank()
            for k in range(8):
                mm(pv_[:, :], XNT[:, k, tsl], WS[4][:, k, :], k == 0, k == 7)
            cp(vxb[:, :, 0:128], pv_[:, :].rearrange("p (h e) -> p h e", e=128), eng="act")
            po_ = bank()
            for k in range(8):
                mm(po_[:, :], XNT[:, k, tsl], WS[1][:, k, :], k == 0, k == 7)
            act(E1, po_[:, :], AF.Tanh, scale=0.5)
            stt(og, E1, 1.0, GHALF, ALU.add, ALU.mult)

        def st_b1(c):
            qt, vxb, pp = QTb[c % 2], VXb[c % 2], PPb[c % 2]
            pO = [bank(), bank()]
            pQ = [bank(), bank()]
            for h in range(4):
                mm(pQ[h // 2][:, (h % 2) * 129:(h % 2) * 129 + 129], qt[:, h, :], CBF[:, h, 0:129])
            for h in range(4):
                mm(pO[h // 2][:, (h % 2) * 129:(h % 2) * 129 + 129], pp[:, h, :], vxb[:, h, 0:129])
            for h in range(4):
                act(QCS[:, h * 129:(h + 1) * 129], pQ[h // 2][:, (h % 2) * 129:(h % 2) * 129 + 129], AF.Identity,
                    scale=INTER[:, c * 4 + h:c * 4 + h + 1])
            for hh in range(2):
                tt(ND[:, hh * 258:(hh + 1) * 258], pO[hh][:, 0:258], QCS[:, hh * 258:(hh + 1) * 258], ALU.add)

        def st_b2a(c):
            og = OGb[c % 2]
            ts(NDEN, ND3[:, :, 128], -1.0, None, ALU.mult)
            tt(DEN, ND3[:, :, 128], NDEN, ALU.max)
            tt(DEN, DEN, v3(FL)[:, c, :], ALU.max)
            recip(RDEN, DEN)
            HV = ND3[:, :, 0:128]
            tt(HV, HV, RDEN.unsqueeze(2).to_broadcast([128, 4, 128]), ALU.mult)
            for h in range(4):
                stt(JNK, ND3[:, h, 0:128], 1.0, ND3[:, h, 0:128], ALU.mult, ALU.mult, accum=SSH[:, h:h + 1])
            rstd_op(RSH, SSH, 1.0 / 128, 4)
            tt(HV, HV, RSH.unsqueeze(2).to_broadcast([128, 4, 128]), ALU.mult)
            tt(YB.rearrange("p (h d) -> p h d", d=128), HV, og.rearrange("p (h d) -> p h d", d=128), ALU.mult)

        def st_b2b(c):
            wkb, vxb = WKb[c % 2], VXb[c % 2]
            pKV = [bank(), bank()]
            for h in range(4):
                mm(pKV[h // 2][:, (h % 2) * 129:(h % 2) * 129 + 129], wkb[:, h, :], vxb[:, h, 0:129])
            for h in range(4):
                stt(CST_[:, h, :], CST_[:, h, :], DEC[:, c * 4 + h:c * 4 + h + 1],
                    pKV[h // 2][:, (h % 2) * 129:(h % 2) * 129 + 129], ALU.mult, ALU.add)
            cp(CBF[:, :, 0:129], CST_, eng="act")

        def st_b2c(c):
            pT = bank()
            pTb = pT[:, :].bitcast(BF16)
            for h in range(4):
                tr(pTb[:, h * 128:(h + 1) * 128], YB[:, h * 128:(h + 1) * 128], IDB)
            cp(YBT, pTb[:, 0:512].rearrange("p (h d) -> p h d", d=128), eng="act")
            for half in range(2):
                po = bank()
                for h in range(4):
                    mm(po[:, :], YBT[:, h, :], WOUT[:, 4 + h, half * 512:(half + 1) * 512], h == 0, h == 3)
                resid_add(c, half, po[:, :])

        st_a(0)
        for c in range(16):
            st_b1(c)
            st_b2b(c)
            st_b2a(c)
            if c + 1 < 16:
                st_a(c + 1)
            st_b2c(c)
    K.even_mixer = even_mixer


    for kind, l in stages:
        if kind == "even":
            norm_to_xnt(gmix_d[l], 0)
            even_mixer(l, l // 2)
        elif kind == "odd":
            norm_to_xnt(gmix_d[l], 0)
            odd_mixer(l // 2)
        elif kind == "ffn":
            norm_to_xnt(gffn_d[l], 1)
            ffn(l)
    yout = y_d.rearrange("(t p) d -> p t d", p=128)
    if final_norm:
        g_b = GB[0]
        dma("sp", "gb0", [(g_b, gfin_d)])
        for t in range(NT):
            act(XS[t % 2], X[:, t, :], AF.Square, accum=SSQ[:, t:t + 1])
        rstd_op(RSTD, SSQ, 1.0 / D, 16)
        for t in range(NT):
            stt(X[:, t, :], X[:, t, :], RSTD[:, t:t + 1], g_b, ALU.mult, ALU.mult)
    for q4 in range(4):
        dma("sp", "yout", [(yout[:, q4 * 4:(q4 + 1) * 4, :], X[:, q4 * 4:(q4 + 1) * 4, :])])
    n_out = P.dma_sems["yout"]
    block = es.enter_context(nc.Block())

    def fin(h):
        h.wait_ge(sems_dma["yout"], n_out)
    P.emit(block, sems_eng, sems_dma, extra={"sp": fin})
    es.close()
    return nc


def _consts():
    s = np.arange(128)[:, None]
    t = np.arange(128)[None, :]
    ident = np.eye(128, dtype=np.float32)
    mst = (s <= t).astype(np.float32)
    ones = np.ones((128, 128), np.float32)
    c_f32 = np.concatenate([ident, mst, ones], axis=1)
    mneg = np.where(s <= t, 0.0, -30000.0).astype(np.float32)
    c_bf = np.concatenate([ident, np.tile(mneg, (1, 4))], axis=1)
    return np.ascontiguousarray(c_f32), np.ascontiguousarray(c_bf)


def make_in_maps(inp):
    f = lambda a: np.ascontiguousarray(np.asarray(a, dtype=np.float32))
    rep = lambda a: np.ascontiguousarray(np.broadcast_to(np.asarray(a, np.float32)[..., None, :], a.shape[:-1] + (128, a.shape[-1])))
    c_f32, c_bf = _consts()
    wc = np.asarray(inp["even_w_conv"], np.float32)
    wconvT = np.ascontiguousarray(wc.reshape(2, 3, 4, 128).transpose(0, 3, 2, 1).reshape(2, 128, 12))
    bs = np.asarray(inp["odd_b_s"], np.float32).reshape(2, 1024)
    shared = {
        "c_f32": c_f32, "c_bf": c_bf,
        "gmix_b": rep(inp["norm_mix"]), "gffn_b": rep(inp["norm_ffn"]), "gfin_b": rep(inp["norm_final"]),
        "bgate_b": rep(inp["even_b_gate"]), "wconvT": wconvT, "ghead_b": rep(inp["even_g_head"]),
        "gv_b": rep(inp["odd_g_v"]), "bs_b": rep(bs),
        "even_w_in": f(inp["even_w_in"]), "even_w_out": f(inp["even_w_out"]),
        "odd_w_in": f(inp["odd_w_in"]), "odd_w_s": f(inp["odd_w_s"]), "odd_w_out": f(inp["odd_w_out"]),
        "ffn_w_in": f(inp["ffn_w_in"]), "ffn_w_out": f(inp["ffn_w_out"]),
    }
    x = np.asarray(inp["x"], np.float32)
    maps = []
    for c in range(8):
        b, sg = c // 4, c % 4
        fl = np.zeros((128, 12), np.float32)
        for j in range(4):
            inc = 1.0 if j < sg else 0.0
            fl[:, j] = inc
            fl[:, 4 + j] = (inc - 1.0) * 1e30
            fl[:, 8 + j] = 1.0 if j == sg - 1 else 0.0
        m = dict(shared)
        m["x"] = np.ascontiguousarray(x[b, sg * TOK:(sg + 1) * TOK])
        m["flags"] = fl
        maps.append(m)
    return maps


def run_stages(inp, stages, final_norm):
    nc = build_program(stages, final_norm)
    maps = make_in_maps(inp)
    res = run_bass_kernel_spmd(nc, maps, core_ids=list(range(8)))
    out = np.empty((2, 8192, 1024), np.float32)
    for c in range(8):
        out[c // 4, (c % 4) * TOK:(c % 4 + 1) * TOK] = res.results[c]["y"]
    return out


def kernel(**inputs):
    stages = []
    for l in range(4):
        stages.append(("even" if l % 2 == 0 else "odd", l))
        stages.append(("ffn", l))
    return run_stages(inputs, stages, True)
```

```python
from contextlib import ExitStack
from concourse.bass_utils import run_bass_kernel_spmd
import numpy as np
import concourse.bass as bass
import concourse.mybir as mybir

F32 = mybir.dt.float32
BF16 = mybir.dt.bfloat16
ALU = mybir.AluOpType
AF = mybir.ActivationFunctionType
AX = mybir.AxisListType


import os
STRICT = bool(os.environ.get('BASS_STRICT'))


class _Op:
    __slots__ = ("eng", "idx", "fn", "waits", "signal", "dma_sem", "dma_cnt", "sigval", "isdma")

    def __init__(self, eng, idx, fn):
        self.eng = eng
        self.idx = idx
        self.fn = fn
        self.waits = []
        self.signal = False
        self.dma_sem = None
        self.dma_cnt = 0
        self.sigval = 0
        self.isdma = False


def _ap_range(ap):
    sp = str(ap.space)
    name = ap.tensor.name
    if "DRAM" in sp.upper() or sp.upper() not in ("SB", "PSUM"):
        return ("D:" + name, 0, 1)
    esz = mybir.dt.size(ap.dtype)
    pat = ap.ap
    pstride = pat[0][0]
    off = ap.offset % pstride if pstride > 0 else ap.offset
    ext = 0
    for st, cnt in pat[1:]:
        ext += abs(st) * (cnt - 1)
    lo = off * esz
    hi = (off + ext + 1) * esz
    if sp.upper() == "PSUM":
        lo, hi = (lo // 2048) * 2048, ((hi + 2047) // 2048) * 2048
    return (sp + ":" + name, lo, hi)


class Prog:
    ENGS = ("pe", "act", "dve", "pool", "sp")

    def __init__(self, nc):
        self.nc = nc
        self.ops = {e: [] for e in self.ENGS}
        self.segs = {}
        self.seen = {e: {f: -1 for f in self.ENGS} for e in self.ENGS}
        self.seen_sem = {e: {} for e in self.ENGS}
        self.dma_sems = {}
        self.extra_sems = []

    def _touch(self, key, lo, hi, op, is_write, deps):
        segs = self.segs.setdefault(key, [])
        new = []
        for s in segs:
            if s[1] <= lo or s[0] >= hi:
                new.append(s)
                continue
            cuts = [s[0]] + [c for c in (lo, hi) if s[0] < c < s[1]] + [s[1]]
            for a, b in zip(cuts[:-1], cuts[1:]):
                new.append([a, b, s[2], dict(s[3])])
        new.sort(key=lambda s: s[0])
        out = []
        cur = lo
        for s in new:
            if s[1] <= lo or s[0] >= hi:
                out.append(s)
                continue
            if s[0] > cur:
                out.append([cur, s[0], None, {}])
            out.append(s)
            cur = s[1]
        if cur < hi:
            out.append([cur, hi, None, {}])
        out.sort(key=lambda s: s[0])
        for s in out:
            if s[1] <= lo or s[0] >= hi:
                continue
            if s[2] is not None:
                deps.append((s[2], "raw" if not is_write else "waw"))
            if is_write:
                for r in s[3].values():
                    deps.append((r, "war"))
                s[2] = op
                s[3] = {}
            else:
                s[3][op.eng if op.dma_sem is None and not op.isdma else ("dma", id(op))] = op
        merged = []
        for s in out:
            if merged and merged[-1][1] == s[0] and merged[-1][2] is s[2] and merged[-1][3] == s[3]:
                merged[-1][1] = s[1]
            else:
                merged.append(s)
        self.segs[key] = merged

    def add(self, eng, fn, reads=(), writes=(), dma_slot=None, n_dma=1):
        op = _Op(eng, len(self.ops[eng]), fn)
        op.isdma = dma_slot is not None
        deps = []
        for ap in reads:
            k, lo, hi = ap if isinstance(ap, tuple) else _ap_range(ap)
            self._touch(k, lo, hi, op, False, deps)
        for ap in writes:
            k, lo, hi = ap if isinstance(ap, tuple) else _ap_range(ap)
            self._touch(k, lo, hi, op, True, deps)
        if dma_slot is not None:
            self.dma_sems[dma_slot] = self.dma_sems.get(dma_slot, 0) + 16 * n_dma
            op.dma_sem = dma_slot
            op.dma_cnt = self.dma_sems[dma_slot]
        best = {}
        for dop, kind in deps:
            if dop is op:
                continue
            if dop.dma_sem is not None:
                cur = self.seen_sem[eng].get(dop.dma_sem, 0)
                if dop.dma_cnt > cur:
                    self.seen_sem[eng][dop.dma_sem] = dop.dma_cnt
                    op.waits.append(("sem", dop.dma_sem, dop.dma_cnt))
                continue
            if dop.eng == eng and not op.isdma:
                if eng == "pe" or (kind != "raw" and not STRICT):
                    continue
            if dop.idx > best.get(dop.eng, -1):
                best[dop.eng] = dop.idx
        for f, idx in best.items():
            if idx > self.seen[eng][f]:
                self.seen[eng][f] = idx
                self.ops[f][idx].signal = True
                op.waits.append(("eng", f, idx))
        self.ops[eng].append(op)
        return op

    def emit(self, block, sems_eng, sems_dma, extra=None):
        nc = self.nc
        for e in self.ENGS:
            c = 0
            for op in self.ops[e]:
                if op.signal:
                    c += 1
                    op.sigval = c
        handles = {"pe": nc.tensor, "act": nc.scalar, "dve": nc.vector, "pool": nc.gpsimd, "sp": nc.sync}

        def body(e):
            def f(h):
                for op in self.ops[e]:
                    for w in op.waits:
                        if w[0] == "sem":
                            h.wait_ge(sems_dma[w[1]], w[2])
                        else:
                            h.wait_ge(sems_eng[w[1]], self.ops[w[1]][w[2]].sigval)
                    ins = op.fn(h)
                    if op.dma_sem is not None:
                        pass
                    elif op.signal:
                        ins.then_inc(sems_eng[e], 1)
                if extra and e in extra:
                    extra[e](h)
            return f
        block.tensor(body("pe"))
        block.scalar(body("act"))
        block.vector(body("dve"))
        block.gpsimd(body("pool"))
        block.sync(body("sp"))


NT = 16
TOK = 2048
D = 1024
EPS = 1e-6
KSC = 128 ** -0.5
SUMW = 532

O_X = 0
O_XNT = 65536
O_CST = 98304
O_GB = O_CST + 8192
O_XS = O_GB + 8192
O_PH = O_XS + 4096
PH_SIZE = 90112
ARENA = O_PH + PH_SIZE


class K:
    pass


def build_program(stages, final_norm=True):
    nc = bass.Bass("TRN2", target_bir_lowering=False)
    P = Prog(nc)
    dt_in = {}

    def din(name, shape):
        t = nc.dram_tensor(name, list(shape), F32, kind="ExternalInput")
        dt_in[name] = t
        return t.ap()

    x_d = din("x", [TOK, D])
    cf_d = din("c_f32", [128, 384])
    cb_d = din("c_bf", [128, 640])
    fl_d = din("flags", [128, 12])
    gmix_d = din("gmix_b", [4, 128, D])
    gffn_d = din("gffn_b", [4, 128, D])
    gfin_d = din("gfin_b", [128, D])
    bgate_d = din("bgate_b", [2, 128, 8])
    wconv_d = din("wconvT", [2, 128, 12])
    ghead_d = din("ghead_b", [2, 128, 512])
    gv_d = din("gv_b", [2, 128, D])
    bs_d = din("bs_b", [2, 128, D])
    ewin_d = din("even_w_in", [2, 1024, 3592])
    ewout_d = din("even_w_out", [2, 1024, 1024])
    owin_d = din("odd_w_in", [2, 1024, 2048])
    ows_d = din("odd_w_s", [2, 8, 128, 128])
    owout_d = din("odd_w_out", [2, 1024, 1024])
    fwin_d = din("ffn_w_in", [4, 1024, 5632])
    fwout_d = din("ffn_w_out", [4, 2816, 1024])
    y_d = nc.dram_tensor("y", [TOK, D], F32, kind="ExternalOutput").ap()
    ccin = [nc.dram_tensor(f"ccin{i}", [128, SUMW], F32) for i in range(2)]
    ccout = [nc.dram_tensor(f"ccout{i}", [512, SUMW], F32) for i in range(2)]

    es = ExitStack()
    A = es.enter_context(nc.sbuf_tensor("arena", [128, ARENA // 4], F32))
    PS = [es.enter_context(nc.psum_tensor(f"ps{i}", [128, 512], F32)) for i in range(8)]
    sems_eng = {e: es.enter_context(nc.semaphore("s_" + e)) for e in Prog.ENGS}
    sems_dma = {}

    def dsem(name):
        if name not in sems_dma:
            sems_dma[name] = es.enter_context(nc.semaphore("d_" + name))
        return sems_dma[name]

    def f32v(off, n, parts=128):
        return A[0:parts, off // 4: off // 4 + n]

    def bfv(off, n, parts=128):
        return A[0:parts, off // 4: off // 4 + (n + 1) // 2].bitcast(BF16)[:, 0:n]

    X = f32v(O_X, NT * D).rearrange("p (t d) -> p t d", d=D)
    XNT = bfv(O_XNT, 8 * TOK).rearrange("p (k t) -> p k t", t=TOK)
    c = O_CST
    IDF = f32v(c, 128); c += 512
    MST = f32v(c, 128); c += 512
    ONES = f32v(c, 128); c += 512
    IDB = bfv(c, 128); c += 256
    MNEG = bfv(c, 512); c += 1024
    FLG = f32v(c, 12); c += 48
    SSQ = f32v(c, 16); c += 64
    RSTD = f32v(c, 16); c += 64
    BGATE = f32v(c, 8); c += 32
    WCONV = f32v(c, 12); c += 48
    SM = f32v(c, 64); c += 256
    ZER = f32v(c, 128); c += 512
    GHEAD = f32v(c, 512); c += 2048
    NEGH = f32v(c, 16); c += 64
    assert c <= O_CST + 8192, c
    GB = [f32v(O_GB + i * 4096, D) for i in range(2)]
    XS = [bfv(O_XS + i * 2048, D) for i in range(2)]

    def mm(out, lhsT, rhs, start=True, stop=True):
        P.add("pe", lambda h, o=out, l=lhsT, r=rhs, s=start, e=stop: h.matmul(o, l, r, start=s, stop=e),
              reads=[lhsT, rhs], writes=[out])

    def tr(out, in_, ident):
        P.add("pe", lambda h, o=out, i=in_, d=ident: h.transpose(o, i, d), reads=[in_, ident], writes=[out])

    def act(out, in_, func, bias=None, scale=None, accum=None):
        rd = [in_] + [v for v in (bias, scale) if v is not None and not isinstance(v, (int, float))]
        wr = [out] + ([accum] if accum is not None else [])
        kw = {}
        if bias is not None:
            kw["bias"] = bias
        if scale is not None:
            kw["scale"] = scale
        if accum is not None:
            kw["accum_out"] = accum
        P.add("act", lambda h, o=out, i=in_, f=func, k=kw: h.activation(out=o, in_=i, func=f, **k), reads=rd, writes=wr)

    def tt(out, in0, in1, op, eng="dve"):
        P.add(eng, lambda h, o=out, a=in0, b=in1, p=op: h.tensor_tensor(out=o, in0=a, in1=b, op=p),
              reads=[in0, in1], writes=[out])

    def ts(out, in0, s1, s2, op0, op1=None, eng="dve"):
        rd = [in0] + [v for v in (s1, s2) if v is not None and not isinstance(v, (int, float))]
        if op1 is None:
            P.add(eng, lambda h, o=out, a=in0, x=s1, p=op0: h.tensor_scalar(out=o, in0=a, scalar1=x, scalar2=None, op0=p),
                  reads=rd, writes=[out])
        else:
            P.add(eng, lambda h, o=out, a=in0, x=s1, y=s2, p=op0, q=op1: h.tensor_scalar(out=o, in0=a, scalar1=x, scalar2=y, op0=p, op1=q),
                  reads=rd, writes=[out])

    def stt(out, in0, scalar, in1, op0, op1, accum=None):
        rd = [in0, in1] + ([scalar] if not isinstance(scalar, (int, float)) else [])
        wr = [out] + ([accum] if accum is not None else [])
        if accum is None:
            P.add("dve", lambda h, o=out, a=in0, s=scalar, b=in1, p=op0, q=op1: h.scalar_tensor_tensor(out=o, in0=a, scalar=s, in1=b, op0=p, op1=q),
                  reads=rd, writes=wr)
        else:
            P.add("dve", lambda h, o=out, a=in0, s=scalar, b=in1, p=op0, q=op1, ac=accum: h.scalar_tensor_tensor(out=o, in0=a, scalar=s, in1=b, op0=p, op1=q, accum_out=ac),
                  reads=rd, writes=wr)

    def cp(out, in_, eng="dve"):
        if eng == "act":
            act(out, in_, AF.Copy)
        else:
            P.add(eng, lambda h, o=out, i=in_: h.tensor_copy(out=o, in_=i), reads=[in_], writes=[out])

    def scan(out, d0, d1, init, op0, op1):
        rd = [d0, d1] + ([init] if not isinstance(init, (int, float)) else [])
        P.add("dve", lambda h, o=out, a=d0, b=d1, i=init, p=op0, q=op1: h.tensor_tensor_scan(out=o, data0=a, data1=b, initial=i, op0=p, op1=q),
              reads=rd, writes=[out])

    def red(out, in_, op):
        P.add("dve", lambda h, o=out, i=in_, p=op: h.tensor_reduce(out=o, in_=i, axis=AX.X, op=p), reads=[in_], writes=[out])

    def recip(out, in_):
        P.add("dve", lambda h, o=out, i=in_: h.reciprocal(out=o, in_=i), reads=[in_], writes=[out])

    def memset(out, val, eng="pool"):
        P.add(eng, lambda h, o=out, v=val: h.memset(o, v), writes=[out])

    def dma(queue, slot, pairs, slot_ap=None, extra_reads=()):
        sem = dsem(slot)
        def fn(h, pairs=pairs, sem=sem):
            r = None
            for o, i in pairs:
                r = h.dma_start(out=o, in_=i).then_inc(sem, 16)
            return r
        wr = [slot_ap] if slot_ap is not None else [o for o, _ in pairs]
        P.add(queue, fn, reads=[i for _, i in pairs] + list(extra_reads), writes=wr, dma_slot=slot, n_dma=len(pairs))

    def rstd_op(out, ss, scale, n):
        ts(out, ss, scale, EPS, ALU.mult, ALU.add)
        P.add("pool", lambda h, o=out, nh=NEGH[:, 0:n]: h.tensor_tensor(out=o, in0=o, in1=nh, op=ALU.pow),
              reads=[out, NEGH[:, 0:n]], writes=[out])

    _bank = [0]

    def bank():
        b = PS[_bank[0] % 8]
        _bank[0] += 1
        return b

    dma("sp", "cst", [(A[:, O_CST // 4: O_CST // 4 + 384], cf_d), (FLG, fl_d)], slot_ap=A[:, O_CST // 4: (O_CST + 8192) // 4])
    dma("pool", "cstb", [(bfv(O_CST + 1536, 640), cb_d)], slot_ap=bfv(O_CST + 1536, 640))
    memset(ZER, 0.0)
    memset(NEGH, -0.5)
    xin = x_d.rearrange("(t p) d -> p t d", p=128)
    for q4 in range(4):
        dma("sp", f"xin{q4}", [(X[:, q4 * 4:(q4 + 1) * 4, :], xin[:, q4 * 4:(q4 + 1) * 4, :])])

    def norm_to_xnt(g_src, gslot):
        g_b = GB[gslot]
        dma("sp", f"gb{gslot}", [(g_b, g_src)])
        for t in range(NT):
            if t % 2 == 0:
                act(XS[0], X[:, t, :], AF.Square, accum=SSQ[:, t:t + 1])
            else:
                stt(XS[1], X[:, t, :], 1.0, X[:, t, :], ALU.mult, ALU.mult, accum=SSQ[:, t:t + 1])
        rstd_op(RSTD, SSQ, 1.0 / D, 16)
        for t in range(NT):
            xs = XS[t % 2]
            stt(xs, X[:, t, :], RSTD[:, t:t + 1], g_b, ALU.mult, ALU.mult)
            pb = bank()
            pbb = pb[:, :].bitcast(BF16)
            for k in range(8):
                tr(pbb[:, k * 128:(k + 1) * 128], xs[:, k * 128:(k + 1) * 128], IDB)
            cp(XNT[:, :, t * 128:(t + 1) * 128], pbb.rearrange("p (k t) -> p k t", t=128), eng="act")

    def resid_add(t, half, ps):
        xv = X[:, t, half * 512:(half + 1) * 512]
        tt(xv, ps, xv, ALU.add)

    def ffn(l):
        groups = [(0, 6), (6, 6), (12, 5), (17, 5)]
        WI = [bfv(O_PH + s * 36864, 8 * 2 * 768).rearrange("p (k u n) -> p k u n", u=2, n=768) for s in range(2)]
        WO = [bfv(O_PH + s * 36864 + 24576, 6 * 1024).rearrange("p (j d) -> p j d", d=1024) for s in range(2)]
        ACTB = [bfv(O_PH + 73728 + s * 6144, 6 * 512).rearrange("p (j t) -> p j t", t=512) for s in range(2)]
        SG = [f32v(O_PH + 86016 + s * 2048, 512) for s in range(2)]
        it = 0
        for gi, (c0, n) in enumerate(groups):
            s = gi % 2
            slot_ap = bfv(O_PH + s * 36864, 18432)
            win = fwin_d[l].rearrange("(k p) n -> p k n", p=128)
            dma("pool", f"ffw{s}", [
                (WI[s][:, :, 0, 0:n * 128], win[:, :, c0 * 128:(c0 + n) * 128]),
                (WI[s][:, :, 1, 0:n * 128], win[:, :, 2816 + c0 * 128:2816 + (c0 + n) * 128]),
                (WO[s][:, 0:n, :], fwout_d[l][c0 * 128:(c0 + n) * 128, :].rearrange("(j p) d -> p j d", p=128)),
            ], slot_ap=slot_ap)
            for b in range(4):
                ab = ACTB[it % 2]
                it += 1
                for j in range(n):
                    pg = bank()
                    pu = bank()
                    for k in range(8):
                        mm(pg[:, :], WI[s][:, k, 0, j * 128:(j + 1) * 128], XNT[:, k, b * 512:(b + 1) * 512], k == 0, k == 7)
                    for k in range(8):
                        mm(pu[:, :], WI[s][:, k, 1, j * 128:(j + 1) * 128], XNT[:, k, b * 512:(b + 1) * 512], k == 0, k == 7)
                    sg = SG[j % 2]
                    act(sg, pg[:, :], AF.Silu)
                    tt(ab[:, j, :], pu[:, :], sg, ALU.mult)
                for i in range(4):
                    t = b * 4 + i
                    for half in range(2):
                        po = bank()
                        for j in range(n):
                            mm(po[:, :], ab[:, j, i * 128:(i + 1) * 128], WO[s][:, j, half * 512:(half + 1) * 512], j == 0, j == n - 1)
                        resid_add(t, half, po[:, :])

    def odd_mixer(j):
        o = O_PH
        WU = bfv(o, 8 * 1024).rearrange("p (k n) -> p k n", n=1024); o += 16384
        WV = bfv(o, 8 * 1024).rearrange("p (k n) -> p k n", n=1024); o += 16384
        WOUT = bfv(o, 8 * 1024).rearrange("p (k n) -> p k n", n=1024); o += 16384
        WST = bfv(o, 8 * 128).rearrange("p (g t) -> p g t", t=128); o += 2048
        BSB = f32v(o, 1024); o += 4096
        GVB = f32v(o, 1024); o += 4096
        UT = bfv(o, 8 * 512).rearrange("p (g t) -> p g t", t=512); o += 8192
        VG = f32v(o, 1024); o += 4096
        VN = bfv(o, 1024); o += 2048
        T1 = f32v(o, 1024); o += 4096
        GT = bfv(o, 1024).rearrange("p (g t) -> p g t", t=128); o += 2048
        WSS = f32v(o, 1024).rearrange("p (g s) -> p g s", s=128); o += 4096
        JNK = bfv(o, 1024); o += 2048
        VNB = bfv(o, 1024); o += 2048
        assert o <= O_PH + PH_SIZE
        UT2 = bfv(O_GB, 8 * 512).rearrange("p (g t) -> p g t", t=512)
        win = owin_d[j].rearrange("(k p) n -> p k n", p=128)
        dma("pool", "owu", [(WU, win[:, :, 0:1024])])
        dma("pool", "owv", [(WV, win[:, :, 1024:2048])])
        dma("pool", "owo", [(WOUT, owout_d[j].rearrange("(k p) n -> p k n", p=128))])
        dma("sp", "ows", [(WSS, ows_d[j].rearrange("g t s -> t g s"))])
        dma("sp", "obs", [(BSB, bs_d[j])])
        dma("sp", "ogv", [(GVB, gv_d[j])])
        for g in range(8):
            pb = bank()
            tr(pb[:, 0:128], WSS[:, g, :], IDF)
            tt(WST[:, g, :], pb[:, 0:128], MST, ALU.mult)
        VN2 = [VN, VNB]
        UTB = [UT, UT2]

        def stage_a(t):
            vn = VN2[t % 2]
            pv = [bank(), bank()]
            for half in range(2):
                for k in range(8):
                    mm(pv[half][:, :], XNT[:, k, t * 128:(t + 1) * 128], WV[:, k, half * 512:(half + 1) * 512], k == 0, k == 7)
            for half in range(2):
                act(VG[:, half * 512:(half + 1) * 512], pv[half][:, :], AF.Gelu_apprx_tanh)
            ss = SM[:, 0:1]
            stt(JNK, VG, 1.0, VG, ALU.mult, ALU.mult, accum=ss)
            rs = SM[:, 1:2]
            rstd_op(rs, ss, 1.0 / 1024, 1)
            stt(vn, VG, rs, GVB, ALU.mult, ALU.mult)

        def stage_b1(t):
            vn = VN2[t % 2]
            b, i = t // 4, t % 4
            ut = UTB[b % 2]
            pg = [bank(), bank()]
            for g in range(8):
                mm(pg[g // 4][:, (g % 4) * 128:(g % 4 + 1) * 128], vn[:, g * 128:(g + 1) * 128], WST[:, g, :])
            for hf in range(2):
                tt(T1[:, hf * 512:(hf + 1) * 512], pg[hf][:, :], BSB[:, hf * 512:(hf + 1) * 512], ALU.add)
            tt(GT, T1.rearrange("p (g t) -> p g t", t=128), ut[:, :, i * 128:(i + 1) * 128], ALU.mult)

        def stage_b2(t):
            for half in range(2):
                po = bank()
                for g in range(8):
                    mm(po[:, :], GT[:, g, :], WOUT[:, g, half * 512:(half + 1) * 512], g == 0, g == 7)
                resid_add(t, half, po[:, :])

        def ublock(b):
            for fc in range(8):
                pb = bank()
                for k in range(8):
                    mm(pb[:, :], WU[:, k, fc * 128:(fc + 1) * 128], XNT[:, k, b * 512:(b + 1) * 512], k == 0, k == 7)
                act(UTB[b % 2][:, fc, :], pb[:, :], AF.Gelu_apprx_tanh)

        ublock(0)
        stage_a(0)
        for t in range(16):
            if t + 1 < 16:
                if (t + 1) % 4 == 0:
                    ublock((t + 1) // 4)
                stage_a(t + 1)
            stage_b1(t)
            stage_b2(t)

    K.norm_to_xnt = norm_to_xnt
    K.ffn = ffn
    K.odd_mixer = odd_mixer
    class Bump:
        def __init__(self, base, limit):
            self.base, self.limit, self.cur = base, limit, base

        def reset(self):
            self.cur = self.base

        def _take(self, nbytes):
            o = self.cur
            self.cur += (nbytes + 31) // 32 * 32
            assert self.cur <= self.limit, (self.cur, self.limit)
            return o

        def f32(self, n, parts=128):
            return f32v(self._take(n * 4), n, parts)

        def bf(self, n, parts=128):
            return bfv(self._take(n * 2), n, parts)

    K.reserved = set()
    _obank = bank

    def bank():
        while True:
            i = _bank[0] % 8
            _bank[0] += 1
            if i not in K.reserved:
                return PS[i]

    def even_mixer(l, j):
        cci = j
        ws_off = [O_PH + i * 8192 for i in range(5)]
        WS = [bfv(ws_off[i], 8 * 512).rearrange("p (k n) -> p k n", n=512) for i in range(5)]
        WOUT = bfv(O_PH + 40960, 8 * 1024).rearrange("p (k n) -> p k n", n=1024)
        sm = Bump(O_PH + 57344, O_PH + 71680)
        tp = Bump(O_PH + 71680, O_PH + PH_SIZE)
        s2 = Bump(ws_off[2], ws_off[2] + 8192)
        win = ewin_d[j].rearrange("(k p) n -> p k n", p=128)

        def loadw(slot, col0):
            dma("pool", f"ew{slot}", [(WS[slot], win[:, :, col0:col0 + 512])])
        loadw(0, 0); loadw(1, 512); loadw(2, 1024)
        dma("pool", "ewo", [(WOUT, ewout_d[j].rearrange("(k p) n -> p k n", p=128))])
        loadw(3, 2048); loadw(4, 2560)
        WG = sm.bf(64).rearrange("p (k n) -> p k n", n=8)
        dma("pool", "ewg", [(WG, win[:, :, 3584:3592])])
        dma("sp", "ebg", [(BGATE, bgate_d[j])])
        dma("sp", "ewc", [(WCONV, wconv_d[j])])
        dma("sp", "egh", [(GHEAD, ghead_d[j])])
        v3 = lambda ap: ap.rearrange("p (c h) -> p c h", h=4)
        ZH = sm.f32(8).rearrange("p (c t) -> p c t", t=2)
        Z01 = sm.f32(8).rearrange("p (c t) -> p c t", t=2)
        BG01 = sm.f32(8).rearrange("p (c t) -> p c t", t=2)
        SUMM = sm.f32(SUMW)
        WC = WCONV.rearrange("p (c j) -> p c j", j=3)
        tp.reset()
        YAB = tp.bf(4 * 512).rearrange("p (c t) -> p c t", t=512)
        XA = tp.f32(512)
        Z = [tp.f32(514) for _ in range(2)]
        ACC = tp.f32(512)
        memset(ZH, 0.0)
        it = 0
        for b in range(4):
            for cc in range(4):
                pbg, pcg, pxa = bank(), bank(), bank()
                for pp, sl in ((pbg, 0), (pcg, 1), (pxa, 2)):
                    for k in range(8):
                        mm(pp[:, :], WS[sl][:, k, cc * 128:(cc + 1) * 128], XNT[:, k, b * 512:(b + 1) * 512], k == 0, k == 7)
                z = Z[it % 2]
                it += 1
                cp(XA, pxa[:, :], eng="act")
                cp(z[:, 0:2], ZH[:, cc, :], eng="pool")
                tt(z[:, 2:514], pcg[:, :], XA, ALU.mult)
                ts(ACC, z[:, 2:514], WC[:, cc, 2:3], None, ALU.mult)
                stt(ACC, z[:, 1:513], WC[:, cc, 1:2], ACC, ALU.mult, ALU.add)
                stt(ACC, z[:, 0:512], WC[:, cc, 0:1], ACC, ALU.mult, ALU.add)
                tt(YAB[:, cc, :], pbg[:, :], ACC, ALU.mult)
                cp(ZH[:, cc, :], z[:, 512:514], eng="pool")
                if b == 0:
                    cp(Z01[:, cc, :], z[:, 2:4], eng="pool")
                    cp(BG01[:, cc, :], pbg[:, 0:2], eng="act")
                if b == 3:
                    cp(SUMM[:, 524 + cc * 2:526 + cc * 2], z[:, 512:514], eng="pool")
            for i in range(4):
                t = b * 4 + i
                for half in range(2):
                    po = bank()
                    for cc in range(4):
                        mm(po[:, :], YAB[:, cc, i * 128:(i + 1) * 128], WOUT[:, cc, half * 512:(half + 1) * 512], cc == 0, cc == 3)
                    resid_add(t, half, po[:, :])
        loadw(0, 1536)
        loadw(1, 3072)
        GC = sm.f32(128).rearrange("p (t n) -> p t n", n=8)
        pgt = bank()
        for t in range(16):
            for k in range(8):
                mm(pgt[:, t * 8:(t + 1) * 8], XNT[:, k, t * 128:(t + 1) * 128], WG[:, k, :], k == 0, k == 7)
        tt(GC, pgt[:, 0:128].rearrange("p (t n) -> p t n", n=8), BGATE.unsqueeze(1).to_broadcast([128, 16, 8]), ALU.add)
        IC = sm.f32(64); LFC = sm.f32(64); EX = sm.f32(64)
        cp(v3(IC), GC[:, :, 0:4])
        act(v3(EX), GC[:, :, 4:8], AF.Exp, scale=-1.0)
        act(EX, EX, AF.Ln, bias=1.0)
        ts(LFC, EX, -1.0, None, ALU.mult)
        pr = bank()
        tr(pr[0:64, 0:128], IC, IDF)
        tr(pr[0:64, 128:256], LFC, IDF)
        IR = sm.f32(128, 64); LFR = sm.f32(128, 64); BR = sm.f32(128, 64); AR = sm.f32(128, 64)
        CMR = sm.f32(128, 64); NEGMR = sm.f32(128, 64)
        cp(IR, pr[0:64, 0:128], eng="act")
        cp(LFR, pr[0:64, 128:256], eng="act")
        scan(BR, LFR, ZER[0:64, :], 0.0, ALU.add, ALU.add)
        tt(AR, IR, BR, ALU.subtract)
        scan(CMR, AR, AR, -1e30, ALU.max, ALU.max)
        pc = bank()
        tr(pc[:, 0:64], AR, IDF[0:64, 0:64])
        tr(pc[:, 64:128], BR, IDF[0:64, 0:64])
        tr(pc[:, 128:192], CMR, IDF[0:64, 0:64])
        ABC = sm.f32(192)
        cp(ABC, pc[:, 0:192], eng="act")
        AC, BC, CMC = ABC[:, 0:64], ABC[:, 64:128], ABC[:, 128:192]
        TB = IR
        TB2 = LFR
        cp(TB, BR[:, 127:128].to_broadcast([64, 128]))
        cp(TB2, CMR[:, 127:128].to_broadcast([64, 128]))
        prp = bank()
        mm(prp[:, 0:64], TB, IDF[0:64, 0:64])
        mm(prp[:, 64:128], TB2, IDF[0:64, 0:64])
        REP = sm.f32(128)
        cp(REP, prp[:, 0:128], eng="act")
        BL, AMX = REP[:, 0:64], REP[:, 64:128]
        BINC = sm.f32(64)
        for h in range(4):
            scan(v3(BINC)[:, :, h], v3(BL)[:, :, h], ZER[:, 0:16], 0.0, ALU.add, ALU.add)
        REM = sm.f32(64)
        tt(REM, BINC, BL, ALU.subtract)
        tt(v3(REM), v3(BINC)[:, 15:16, :].to_broadcast([128, 16, 4]), v3(REM), ALU.subtract)
        VAL = sm.f32(64)
        tt(VAL, AMX, REM, ALU.add)
        MLOC = sm.f32(4)
        red(MLOC, VAL.rearrange("p (c h) -> p h c", h=4), ALU.max)
        TT_ = sm.f32(64)
        tt(v3(TT_), v3(REM), MLOC.unsqueeze(1).to_broadcast([128, 16, 4]), ALU.subtract)
        WEND = sm.f32(64)
        tt(WEND, AC, TT_, ALU.add)
        act(WEND, WEND, AF.Exp)
        tp.reset()
        WKA = [tp.bf(512).rearrange("p (h d) -> p h d", d=128) for _ in range(2)]
        VEX = [tp.bf(520).rearrange("p (h e) -> p h e", e=130) for _ in range(2)]
        for vv in VEX:
            memset(vv[:, :, 128:130], 1.0)
        K.reserved = {4, 5, 6, 7}
        CA = [PS[4 + h] for h in range(4)]
        for c in range(16):
            tsl = slice(c * 128, (c + 1) * 128)
            pk, pv_ = bank(), bank()
            for k in range(8):
                mm(pk[:, :], XNT[:, k, tsl], WS[3][:, k, :], k == 0, k == 7)
            for k in range(8):
                mm(pv_[:, :], XNT[:, k, tsl], WS[4][:, k, :], k == 0, k == 7)
            wk = WKA[c % 2]
            vx = VEX[c % 2]
            stt(wk, pk[:, :].rearrange("p (h d) -> p h d", d=128), KSC,
                v3(WEND)[:, c, :].unsqueeze(2).to_broadcast([128, 4, 128]), ALU.mult, ALU.mult)
            cp(vx[:, :, 0:128], pv_[:, :].rearrange("p (h e) -> p h e", e=128), eng="act")
            for h in range(4):
                mm(CA[h][:, 0:129], wk[:, h, :], vx[:, h, 0:129], c == 0, c == 15)
        for h in range(4):
            cp(SUMM[:, h * 129:(h + 1) * 129], CA[h][:, 0:129], eng="act")
        K.reserved = set()
        cp(SUMM[:, 516:520], MLOC)
        cp(SUMM[:, 520:524], v3(BINC)[:, 15, :])
        dma("sp", f"ccin{cci}", [(ccin[cci].ap(), SUMM)])
        csem = dsem(f"cc{cci}")

        def ccfn(h, cci=cci, csem=csem):
            return h.collective_compute("AllGather", ALU.bypass, replica_groups=[[0, 1, 2, 3], [4, 5, 6, 7]],
                                        ins=[ccin[cci].ap().opt()], outs=[ccout[cci].ap().opt()]).then_inc(csem)
        _cop = P.add("pool", ccfn, reads=[ccin[cci].ap()], writes=[ccout[cci].ap()], dma_slot=f"cc{cci}", n_dma=1)
        _cop.dma_cnt = 1
        P.dma_sems[f"cc{cci}"] = 1
        s2.reset()
        G = s2.f32(SUMW)
        CST_ = s2.f32(516).rearrange("p (h e) -> p h e", e=129)
        CBF = s2.bf(520).rearrange("p (h e) -> p h e", e=130)
        TMPC = s2.f32(129)
        MREP = sm.f32(4); HZ = sm.f32(8)
        BJ = sm.f32(4); MJ = sm.f32(4); T1_ = sm.f32(4); MN = sm.f32(4); S12 = sm.f32(8)
        memset(CST_, 0.0)
        memset(MREP, 0.0)
        memset(HZ, 0.0)
        for jr in range(3):
            dma("sp", "gload", [(G, ccout[cci].ap()[jr * 128:(jr + 1) * 128, :])])
            inc = FLG[:, jr:jr + 1]; neg = FLG[:, 4 + jr:5 + jr]; prv = FLG[:, 8 + jr:9 + jr]
            ts(BJ, G[:, 520:524], inc, None, ALU.mult)
            ts(MJ, G[:, 516:520], inc, neg, ALU.mult, ALU.add)
            tt(T1_, MREP, BJ, ALU.add)
            tt(MN, T1_, MJ, ALU.max)
            tt(S12[:, 0:4], T1_, MN, ALU.subtract)
            tt(S12[:, 4:8], MJ, MN, ALU.subtract)
            act(S12, S12, AF.Exp)
            for h in range(4):
                ts(TMPC, G[:, h * 129:(h + 1) * 129], S12[:, 4 + h:5 + h], None, ALU.mult)
                stt(CST_[:, h, :], CST_[:, h, :], S12[:, h:h + 1], TMPC, ALU.mult, ALU.add)
            cp(MREP, MN)
            stt(HZ, G[:, 524:532], prv, HZ, ALU.mult, ALU.add)
        cp(CBF[:, :, 0:129], CST_, eng="act")
        tp.reset()
        QT = tp.bf(512).rearrange("p (h d) -> p h d", d=128)
        KT = tp.bf(512).rearrange("p (h d) -> p h d", d=128)
        WK = tp.bf(512).rearrange("p (h d) -> p h d", d=128)
        VX = tp.bf(520).rearrange("p (h e) -> p h e", e=130)
        OG = tp.bf(512)
        RC = tp.f32(512, 64)
        DD = tp.bf(512).rearrange("p (h d) -> p h d", d=128)
        PP = tp.bf(512).rearrange("p (h d) -> p h d", d=128)
        QCS = tp.f32(516)
        ND = tp.f32(516)
        JNK = tp.bf(128)
        YB = tp.bf(512)
        YBT = tp.bf(512).rearrange("p (h d) -> p h d", d=128)
        DY = tp.bf(512).rearrange("p (c t) -> p c t", t=128)
        DEN = tp.f32(4); NDEN = tp.f32(4); RDEN = tp.f32(4); SSH = tp.f32(4); RSH = tp.f32(4)
        D0 = tp.f32(4); D1 = tp.f32(4); D2 = tp.f32(4)
        HZ3 = HZ.rearrange("p (c t) -> p c t", t=2)
        tt(D0, HZ3[:, :, 0], WC[:, :, 0], ALU.mult)
        tt(D2, HZ3[:, :, 1], WC[:, :, 1], ALU.mult)
        tt(D0, D0, D2, ALU.add)
        tt(D0, D0, BG01[:, :, 0], ALU.mult)
        tt(D1, HZ3[:, :, 1], WC[:, :, 0], ALU.mult)
        tt(D1, D1, BG01[:, :, 1], ALU.mult)
        memset(DY, 0.0)
        cp(DY[:, :, 0], D0)
        cp(DY[:, :, 1], D1)
        for half in range(2):
            po = bank()
            for cc in range(4):
                mm(po[:, :], DY[:, cc, :], WOUT[:, cc, half * 512:(half + 1) * 512], cc == 0, cc == 3)
            resid_add(0, half, po[:, :])
        MNEXT = sm.f32(64); MCUR = sm.f32(64); MX = sm.f32(64); DEC = sm.f32(64); WKB = sm.f32(64)
        MCOL = sm.f32(64); INTER = sm.f32(64); FL = sm.f32(64)
        for h in range(4):
            scan(v3(MNEXT)[:, :, h], v3(AMX)[:, :, h], v3(BL)[:, :, h], MREP[:, h:h + 1], ALU.max, ALU.add)
        cp(v3(MCUR)[:, 0, :], MREP)
        cp(v3(MCUR)[:, 1:16, :], v3(MNEXT)[:, 0:15, :])
        tt(MX, MCUR, AMX, ALU.max)
        tt(DEC, MCUR, MX, ALU.subtract)
        act(DEC, DEC, AF.Exp)
        tt(WKB, AC, MX, ALU.subtract)
        act(WKB, WKB, AF.Exp)
        tt(MCOL, CMC, MCUR, ALU.max)
        tt(INTER, MCUR, MCOL, ALU.subtract)
        act(INTER, INTER, AF.Exp)
        tt(FL, BC, MCOL, ALU.add)
        act(FL, FL, AF.Exp, scale=-1.0)
        T64 = AR[:, 0:64]
        MR = sm.f32(1, 64)
        tt(T64, MCUR[0:64, :], IDF[0:64, 0:64], ALU.mult)
        red(MR, T64, ALU.add)
        ts(NEGMR, CMR, MR, -1.0, ALU.max, ALU.mult)
        xb = Bump(O_GB, O_PH)
        QTb = [QT, xb.bf(512).rearrange("p (h d) -> p h d", d=128)]
        WKb = [WK, xb.bf(512).rearrange("p (h d) -> p h d", d=128)]
        VXb = [VX, xb.bf(520).rearrange("p (h e) -> p h e", e=130)]
        OGb = [OG, xb.bf(512)]
        PPb = [PP, xb.bf(512).rearrange("p (h d) -> p h d", d=128)]
        E1 = xb.f32(512)
        GHALF = xb.f32(512)
        ts(GHALF, GHEAD, 0.5, None, ALU.mult)
        for vv in VXb:
            memset(vv[:, :, 128:130], 1.0)
        ND3 = ND.rearrange("p (h e) -> p h e", e=129)

        def st_rc(c):
            tt(RC.rearrange("p (h t) -> p h t", t=128), NEGMR.unsqueeze(1).to_broadcast([64, 4, 128]),
               IDF[0:64, 4 * c:4 * c + 4].unsqueeze(2).to_broadcast([64, 4, 128]), ALU.mult)

        def st_a(c):
            tsl = slice(c * 128, (c + 1) * 128)
            qt, wkb, vxb, og, pp = QTb[c % 2], WKb[c % 2], VXb[c % 2], OGb[c % 2], PPb[c % 2]
            pq = bank()
            for h in range(4):
                for k in range(8):
                    mm(pq[:, h * 128:(h + 1) * 128], WS[0][:, k, h * 128:(h + 1) * 128], XNT[:, k, tsl], k == 0, k == 7)
            cp(qt, pq[:, :].rearrange("p (h d) -> p h d", d=128), eng="act")
            pkk = bank()
            for h in range(4):
                for k in range(8):
                    mm(pkk[:, h * 128:(h + 1) * 128], WS[3][:, k, h * 128:(h + 1) * 128], XNT[:, k, tsl], k == 0, k == 7)
            cp(KT, pkk[:, :].rearrange("p (h d) -> p h d", d=128), eng="act")
            pkt = bank()
            for k in range(8):
                mm(pkt[:, :], XNT[:, k, tsl], WS[3][:, k, :], k == 0, k == 7)
            pv_ = bank()
            for k in range(8):
                mm(pv_[:, :], XNT[:, k, tsl], WS[4][:, k, :], k == 0, k == 7)
            po_ = bank()
            for k in range(8):
                mm(po_[:, :], XNT[:, k, tsl], WS[1][:, k, :], k == 0, k == 7)
            pe_ = bank()
            mm(pe_[:, :], IDB, MNEG, True, False)
            mm(pe_[:, :], ONES[0:64, :], RC, False, True)
            ps_ = bank()
            for h in range(4):
                mm(ps_[:, h * 128:(h + 1) * 128], KT[:, h, :], qt[:, h, :])
            stt(wkb, pkt[:, :].rearrange("p (h d) -> p h d", d=128), KSC,
                v3(WKB)[:, c, :].unsqueeze(2).to_broadcast([128, 4, 128]), ALU.mult, ALU.mult)
            cp(vxb[:, :, 0:128], pv_[:, :].rearrange("p (h e) -> p h e", e=128), eng="act")
            act(E1, po_[:, :], AF.Tanh, scale=0.5)
            stt(og, E1, 1.0, GHALF, ALU.add, ALU.mult)
            for h in range(4):
                act(DD[:, h, :], pe_[:, h * 128:(h + 1) * 128], AF.Exp, bias=AC[:, c * 4 + h:c * 4 + h + 1])
            stt(pp, ps_[:, :].rearrange("p (h d) -> p h d", d=128), KSC, DD, ALU.mult, ALU.mult)

        def st_b1(c):
            qt, vxb, pp = QTb[c % 2], VXb[c % 2], PPb[c % 2]
            pO = [bank(), bank()]
            pQ = [bank(), bank()]
            for h in range(4):
                mm(pQ[h // 2][:, (h % 2) * 129:(h % 2) * 129 + 129], qt[:, h, :], CBF[:, h, 0:129])
            for h in range(4):
                mm(pO[h // 2][:, (h % 2) * 129:(h % 2) * 129 + 129], pp[:, h, :], vxb[:, h, 0:129])
            for h in range(4):
                act(QCS[:, h * 129:(h + 1) * 129], pQ[h // 2][:, (h % 2) * 129:(h % 2) * 129 + 129], AF.Identity,
                    scale=INTER[:, c * 4 + h:c * 4 + h + 1])
            for hh in range(2):
                tt(ND[:, hh * 258:(hh + 1) * 258], pO[hh][:, 0:258], QCS[:, hh * 258:(hh + 1) * 258], ALU.add)

        def st_b2a(c):
            og = OGb[c % 2]
            ts(NDEN, ND3[:, :, 128], -1.0, None, ALU.mult)
            tt(DEN, ND3[:, :, 128], NDEN, ALU.max)
            tt(DEN, DEN, v3(FL)[:, c, :], ALU.max)
            recip(RDEN, DEN)
            HV = ND3[:, :, 0:128]
            tt(HV, HV, RDEN.unsqueeze(2).to_broadcast([128, 4, 128]), ALU.mult)
            for h in range(4):
                stt(JNK, ND3[:, h, 0:128], 1.0, ND3[:, h, 0:128], ALU.mult, ALU.mult, accum=SSH[:, h:h + 1])
            rstd_op(RSH, SSH, 1.0 / 128, 4)
            tt(HV, HV, RSH.unsqueeze(2).to_broadcast([128, 4, 128]), ALU.mult)
            tt(YB.rearrange("p (h d) -> p h d", d=128), HV, og.rearrange("p (h d) -> p h d", d=128), ALU.mult)

        def st_b2b(c):
            wkb, vxb = WKb[c % 2], VXb[c % 2]
            pKV = [bank(), bank()]
            for h in range(4):
                mm(pKV[h // 2][:, (h % 2) * 129:(h % 2) * 129 + 129], wkb[:, h, :], vxb[:, h, 0:129])
            for h in range(4):
                stt(CST_[:, h, :], CST_[:, h, :], DEC[:, c * 4 + h:c * 4 + h + 1],
                    pKV[h // 2][:, (h % 2) * 129:(h % 2) * 129 + 129], ALU.mult, ALU.add)
            cp(CBF[:, :, 0:129], CST_, eng="act")

        def st_b2c(c):
            pT = bank()
            pTb = pT[:, :].bitcast(BF16)
            for h in range(4):
                tr(pTb[:, h * 128:(h + 1) * 128], YB[:, h * 128:(h + 1) * 128], IDB)
            cp(YBT, pTb[:, 0:512].rearrange("p (h d) -> p h d", d=128), eng="act")
            for half in range(2):
                po = bank()
                for h in range(4):
                    mm(po[:, :], YBT[:, h, :], WOUT[:, 4 + h, half * 512:(half + 1) * 512], h == 0, h == 3)
                resid_add(c, half, po[:, :])

        st_rc(0)
        st_a(0)
        for c in range(16):
            st_b1(c)
            if c + 1 < 16:
                st_rc(c + 1)
            st_b2b(c)
            st_b2a(c)
            if c + 1 < 16:
                st_a(c + 1)
            st_b2c(c)
    K.even_mixer = even_mixer


    for kind, l in stages:
        if kind == "even":
            norm_to_xnt(gmix_d[l], 0)
            even_mixer(l, l // 2)
        elif kind == "odd":
            norm_to_xnt(gmix_d[l], 0)
            odd_mixer(l // 2)
        elif kind == "ffn":
            norm_to_xnt(gffn_d[l], 1)
            ffn(l)
    yout = y_d.rearrange("(t p) d -> p t d", p=128)
    if final_norm:
        g_b = GB[0]
        dma("sp", "gb0", [(g_b, gfin_d)])
        for t in range(NT):
            act(XS[t % 2], X[:, t, :], AF.Square, accum=SSQ[:, t:t + 1])
        rstd_op(RSTD, SSQ, 1.0 / D, 16)
        for t in range(NT):
            stt(X[:, t, :], X[:, t, :], RSTD[:, t:t + 1], g_b, ALU.mult, ALU.mult)
    for q4 in range(4):
        dma("sp", "yout", [(yout[:, q4 * 4:(q4 + 1) * 4, :], X[:, q4 * 4:(q4 + 1) * 4, :])])
    n_out = P.dma_sems["yout"]
    block = es.enter_context(nc.Block())

    def fin(h):
        h.wait_ge(sems_dma["yout"], n_out)
    P.emit(block, sems_eng, sems_dma, extra={"sp": fin})
    es.close()
    return nc


def _consts():
    s = np.arange(128)[:, None]
    t = np.arange(128)[None, :]
    ident = np.eye(128, dtype=np.float32)
    mst = (s <= t).astype(np.float32)
    ones = np.ones((128, 128), np.float32)
    c_f32 = np.concatenate([ident, mst, ones], axis=1)
    mneg = np.where(s <= t, 0.0, -30000.0).astype(np.float32)
    c_bf = np.concatenate([ident, np.tile(mneg, (1, 4))], axis=1)
    return np.ascontiguousarray(c_f32), np.ascontiguousarray(c_bf)


def make_in_maps(inp):
    f = lambda a: np.ascontiguousarray(np.asarray(a, dtype=np.float32))
    rep = lambda a: np.ascontiguousarray(np.broadcast_to(np.asarray(a, np.float32)[..., None, :], a.shape[:-1] + (128, a.shape[-1])))
    c_f32, c_bf = _consts()
    wc = np.asarray(inp["even_w_conv"], np.float32)
    wconvT = np.ascontiguousarray(wc.reshape(2, 3, 4, 128).transpose(0, 3, 2, 1).reshape(2, 128, 12))
    bs = np.asarray(inp["odd_b_s"], np.float32).reshape(2, 1024)
    shared = {
        "c_f32": c_f32, "c_bf": c_bf,
        "gmix_b": rep(inp["norm_mix"]), "gffn_b": rep(inp["norm_ffn"]), "gfin_b": rep(inp["norm_final"]),
        "bgate_b": rep(inp["even_b_gate"]), "wconvT": wconvT, "ghead_b": rep(inp["even_g_head"]),
        "gv_b": rep(inp["odd_g_v"]), "bs_b": rep(bs),
        "even_w_in": f(inp["even_w_in"]), "even_w_out": f(inp["even_w_out"]),
        "odd_w_in": f(inp["odd_w_in"]), "odd_w_s": f(inp["odd_w_s"]), "odd_w_out": f(inp["odd_w_out"]),
        "ffn_w_in": f(inp["ffn_w_in"]), "ffn_w_out": f(inp["ffn_w_out"]),
    }
    x = np.asarray(inp["x"], np.float32)
    maps = []
    for c in range(8):
        b, sg = c // 4, c % 4
        fl = np.zeros((128, 12), np.float32)
        for j in range(4):
            inc = 1.0 if j < sg else 0.0
            fl[:, j] = inc
            fl[:, 4 + j] = (inc - 1.0) * 1e30
            fl[:, 8 + j] = 1.0 if j == sg - 1 else 0.0
        m = dict(shared)
        m["x"] = np.ascontiguousarray(x[b, sg * TOK:(sg + 1) * TOK])
        m["flags"] = fl
        maps.append(m)
    return maps


def run_stages(inp, stages, final_norm):
    nc = build_program(stages, final_norm)
    maps = make_in_maps(inp)
    res = run_bass_kernel_spmd(nc, maps, core_ids=list(range(8)))
    out = np.empty((2, 8192, 1024), np.float32)
    for c in range(8):
        out[c // 4, (c % 4) * TOK:(c % 4 + 1) * TOK] = res.results[c]["y"]
    return out


def kernel(**inputs):
    stages = []
    for l in range(4):
        stages.append(("even" if l % 2 == 0 else "odd", l))
        stages.append(("ffn", l))
    return run_stages(inputs, stages, True)
```
